# Optimizing a Trainium2 kernel written in Bass

```python
import math
import jax, jax.numpy as jnp
from jax import lax
import numpy as np

D_MODEL = 2048
BATCH = 8
SEQ = 4096
DEPTH = 2

F32 = jnp.float32
EPS = 1e-6
D_MIX = D_MODEL
GDN_HEADS = 8
GDN_DK = 128
GDN_DV = 128
GDN_W = GDN_HEADS * GDN_DV
GDN_CONV = 3
GDN_CHUNK = 64
HY_GROUPS = 4
HY_W = D_MIX // 4
HY_CONV = 3
HY_EMB = 33
HY_BANDS = (HY_EMB - 1) // 2
HY_FFN = 64
HY_STEEP_DECAY_PCT = 0.3
HY_SHALLOW_DECAY_PCT = 1.5
HY_DECAY_TARGET = 1e-2
FN_GROUPS = 4
FN_W = D_MIX - GDN_W - HY_W
FN_GC = FN_W // FN_GROUPS
OFF_Z = 3 * GDN_W
OFF_BA = 4 * GDN_W
OFF_HY = OFF_BA + 4 * GDN_HEADS
OFF_FN = OFF_HY + 3 * HY_W
IN_COLS = OFF_FN + FN_W
PEER_HEADS = 8
PEER_NKEYS = 128
PEER_N = PEER_NKEYS * PEER_NKEYS
PEER_QDIM = 256
PEER_HALF = PEER_QDIM // 2
PEER_TOPK = 16
PEER_BLOCK = 128

kernel_name = 'hybrid_gdn_hyena_fnet_peer_encoder'


def rms_norm(x, w):
    xf = x.astype(F32)
    y = xf * lax.rsqrt(jnp.mean(xf * xf, axis=-1, keepdims=True) + EPS)
    return (y * w.astype(F32)).astype(x.dtype)


def depthwise_conv_centred(x, w):
    c = x.shape[-1]
    return lax.conv_general_dilated(x, w[:, None, :].astype(x.dtype), window_strides=(1,), padding='SAME',
                                    dimension_numbers=('NWC', 'WIO', 'NWC'), feature_group_count=c)


def l2_normalize(t):
    return t * lax.rsqrt(jnp.sum(t * t, axis=-1, keepdims=True) + EPS)


def gated_delta_chunked(q, k, v, g, beta):
    b, h, l, dk = q.shape
    dv = v.shape[-1]
    c = GDN_CHUNK
    n = l // c
    q = q.reshape(b, h, n, c, dk)
    k = k.reshape(b, h, n, c, dk)
    v = v.reshape(b, h, n, c, dv)
    g = jnp.cumsum(g.reshape(b, h, n, c), axis=-1)
    beta = beta.reshape(b, h, n, c)
    incl = jnp.tril(jnp.ones((c, c), dtype=bool))
    strict = jnp.tril(jnp.ones((c, c), dtype=bool), k=-1)
    diff = g[..., :, None] - g[..., None, :]
    decay = jnp.where(incl, jnp.exp(jnp.where(incl, diff, 0.0)), 0.0)
    k_beta = k * beta[..., None]
    a = jnp.where(strict, jnp.einsum('bhnid,bhnjd->bhnij', k_beta, k) * decay, 0.0)
    t_mat = a + jnp.eye(c, dtype=a.dtype)
    rhs = jnp.concatenate([v * beta[..., None], k_beta * jnp.exp(g)[..., None]], axis=-1)
    sol = lax.linalg.triangular_solve(t_mat, rhs, left_side=True, lower=True, unit_diagonal=True)
    u_c, w_c = sol[..., :dv], sol[..., dv:]
    qk = jnp.einsum('bhnid,bhnjd->bhnij', q, k) * decay

    def step(state, inp):
        q_i, k_i, u_i, w_i, g_i, qk_i = inp
        v_new = u_i - jnp.einsum('bhcd,bhde->bhce', w_i, state)
        o = (jnp.einsum('bhcd,bhde->bhce', q_i * jnp.exp(g_i)[..., None], state)
             + jnp.einsum('bhcj,bhje->bhce', qk_i, v_new))
        g_last = g_i[..., -1:]
        state = (state * jnp.exp(g_last)[..., None]
                 + jnp.einsum('bhcd,bhce->bhde', k_i * jnp.exp(g_last - g_i)[..., None], v_new))
        return state, o

    xs = tuple(jnp.moveaxis(t, 2, 0) for t in (q, k, u_c, w_c, g, qk))
    state0 = jnp.zeros((b, h, dk, dv), F32)
    _, o = lax.scan(step, state0, xs)
    return jnp.moveaxis(o, 0, 2).reshape(b, h, l, dv)


def gdn_mixer(p, conv_w, a_log, dt_bias, norm_w):
    b, s, _ = p.shape
    qkv = jax.nn.silu(depthwise_conv_centred(p[..., :OFF_Z], conv_w)).astype(F32)
    q, k, v = jnp.split(qkv, 3, axis=-1)
    q = l2_normalize(q.reshape(b, s, GDN_HEADS, GDN_DK)) * (GDN_DK ** -0.5)
    k = l2_normalize(k.reshape(b, s, GDN_HEADS, GDN_DK))
    v = v.reshape(b, s, GDN_HEADS, GDN_DV)
    z = p[..., OFF_Z:OFF_BA].astype(F32).reshape(b, s, GDN_HEADS, GDN_DV)
    ba = p[..., OFF_BA:OFF_HY].astype(F32).reshape(b, s, 2, 2, GDN_HEADS)
    beta = jax.nn.sigmoid(ba[:, :, 0])
    g = -jnp.exp(a_log.astype(F32)) * jax.nn.softplus(ba[:, :, 1] + dt_bias.astype(F32))
    qh, kh, vh = (jnp.transpose(t, (0, 2, 1, 3)) for t in (q, k, v))
    gh = jnp.transpose(g, (0, 2, 3, 1))
    bh = jnp.transpose(beta, (0, 2, 3, 1))
    rev = lambda t: jnp.flip(t, axis=2)
    o_fwd = gated_delta_chunked(qh, kh, vh, gh[:, 0], bh[:, 0])
    o_bwd = rev(gated_delta_chunked(rev(qh), rev(kh), rev(vh), rev(gh[:, 1]), rev(bh[:, 1])))
    o = jnp.transpose(o_fwd + o_bwd, (0, 2, 1, 3))
    o = o * lax.rsqrt(jnp.mean(o * o, axis=-1, keepdims=True) + EPS) * norm_w.astype(F32) * jax.nn.silu(z)
    return o.reshape(b, s, GDN_W).astype(p.dtype)


def hyena_filters(l, w1, b1, w2, b2, w3, b3, w4, b4, freq):
    t = jnp.linspace(0.0, 1.0, l, dtype=F32)[:, None]
    ang = (2.0 * math.pi / l) * jnp.arange(l, dtype=F32)[:, None]
    f = jnp.linspace(1e-4, HY_BANDS - 1, HY_BANDS, dtype=F32)[None, :]
    z = jnp.concatenate([t, jnp.cos(f * ang), -jnp.sin(f * ang)], axis=-1)
    fr = freq.astype(F32)
    hid = jnp.sin(fr * (z @ w1.astype(F32) + b1.astype(F32)))
    hid = jnp.sin(fr * (hid @ w2.astype(F32) + b2.astype(F32)))
    hid = jnp.sin(fr * (hid @ w3.astype(F32) + b3.astype(F32)))
    h = hid @ w4.astype(F32) + b4.astype(F32)
    max_decay = math.log(HY_DECAY_TARGET) / HY_STEEP_DECAY_PCT
    min_decay = math.log(HY_DECAY_TARGET) / HY_SHALLOW_DECAY_PCT
    deltas = jnp.linspace(min_decay, max_decay, HY_W, dtype=F32)
    window = jnp.exp(-t * jnp.abs(deltas)[None, :])
    h_fwd = h[:, :HY_W] * window
    h_bwd = h[:, HY_W:] * window
    buf = jnp.concatenate([h_fwd, jnp.zeros((1, HY_W), F32), h_bwd[:0:-1]], axis=0)
    return buf / jnp.sum(jnp.abs(buf), axis=0, keepdims=True)


def hyena_mixer(p, conv_w, conv_b, w1, b1, w2, b2, w3, b3, w4, b4, freq, skip):
    b, l, _ = p.shape
    u = (depthwise_conv_centred(p[..., OFF_HY:OFF_FN], conv_w) + conv_b.astype(p.dtype)).astype(F32)
    x1, x2, v = jnp.split(u, 3, axis=-1)
    filt = hyena_filters(l, w1, b1, w2, b2, w3, b3, w4, b4, freq)
    n = 2 * l
    vg = v * x2
    y = jnp.fft.irfft(jnp.fft.rfft(vg, n=n, axis=1) * jnp.fft.rfft(filt, n=n, axis=0)[None], n=n, axis=1)[:, :l]
    y = (y + vg * skip.astype(F32)) * x1
    return y.astype(p.dtype)


def fnet_mixer(p, w):
    b, s, _ = p.shape
    a = p[..., OFF_FN:].astype(F32).reshape(b, s, FN_GROUPS, FN_GC)
    mixed = jnp.real(jnp.fft.fft2(a, axes=(1, 3), norm='ortho'))
    y = jnp.einsum('bsgc,gce->bsge', mixed, w.astype(F32))
    return y.reshape(b, s, FN_W).astype(p.dtype)


def peer_ffn(h, wq, k1, k2, u_tab, v_tab):
    b, s, d = h.shape
    q = jnp.einsum('bsd,de->bse', h, wq).reshape(b, s, PEER_HEADS, 2, PEER_HALF)
    s1 = jnp.einsum('bshd,nd->bshn', q[..., 0, :], k1).astype(F32)
    s2 = jnp.einsum('bshd,nd->bshn', q[..., 1, :], k2).astype(F32)
    v1, i1 = lax.top_k(s1, PEER_TOPK)
    v2, i2 = lax.top_k(s2, PEER_TOPK)
    cand = (v1[..., :, None] + v2[..., None, :]).reshape(b, s, PEER_HEADS, PEER_TOPK * PEER_TOPK)
    sc, ci = lax.top_k(cand, PEER_TOPK)
    e1 = jnp.take_along_axis(i1, ci // PEER_TOPK, axis=-1)
    e2 = jnp.take_along_axis(i2, ci % PEER_TOPK, axis=-1)
    expert = (e1 * PEER_NKEYS + e2).reshape(-1, PEER_BLOCK, PEER_HEADS * PEER_TOPK)
    gate = jax.nn.softmax(sc, axis=-1).astype(h.dtype).reshape(-1, PEER_BLOCK, PEER_HEADS * PEER_TOPK)
    tokens = h.reshape(-1, PEER_BLOCK, d)

    def block(args):
        hb, eb, gb = args
        act = jax.nn.gelu(jnp.einsum('td,ted->te', hb, u_tab[eb]), approximate=False)
        return jnp.einsum('te,ted->td', act * gb, v_tab[eb])

    out = lax.map(block, (tokens, expert, gate))
    return out.reshape(b, s, d)


def setup_inputs(seed: int = 0) -> dict:
    key = jax.random.key(seed)
    ks = iter(jax.random.split(key, 40))
    nrm = lambda shape, scale: jax.random.normal(next(ks), shape, F32) * scale
    gain = lambda shape: 1.0 + nrm(shape, 0.01)
    L = DEPTH
    a_log = jnp.log(jax.random.uniform(next(ks), (L, 2, GDN_HEADS), F32, 1.0, 16.0))
    dt = jnp.exp(jax.random.uniform(next(ks), (L, 2, GDN_HEADS), F32, math.log(1e-3), math.log(0.1)))
    dt_bias = dt + jnp.log(-jnp.expm1(-dt))
    return {
        'x': nrm((BATCH, SEQ, D_MODEL), 1.0),
        'norm1_w': gain((L, D_MODEL)),
        'w_in': nrm((L, D_MODEL, IN_COLS), D_MODEL ** -0.5),
        'gdn_conv_w': nrm((L, GDN_CONV, 3 * GDN_W), GDN_CONV ** -0.5),
        'gdn_a_log': a_log,
        'gdn_dt_bias': dt_bias,
        'gdn_norm_w': gain((L, GDN_DV)),
        'hy_conv_w': nrm((L, HY_CONV, 3 * HY_W), HY_CONV ** -0.5),
        'hy_conv_b': nrm((L, 3 * HY_W), 0.02),
        'hy_w1': nrm((L, HY_EMB, HY_FFN), HY_EMB ** -0.5),
        'hy_b1': nrm((L, HY_FFN), 0.02),
        'hy_w2': nrm((L, HY_FFN, HY_FFN), HY_FFN ** -0.5),
        'hy_b2': nrm((L, HY_FFN), 0.02),
        'hy_w3': nrm((L, HY_FFN, HY_FFN), HY_FFN ** -0.5),
        'hy_b3': nrm((L, HY_FFN), 0.02),
        'hy_w4': nrm((L, HY_FFN, 2 * HY_W), HY_FFN ** -0.5),
        'hy_b4': nrm((L, 2 * HY_W), 0.02),
        'hy_freq': gain((L, HY_FFN)),
        'hy_skip': nrm((L, HY_W), 0.1),
        'fnet_w': nrm((L, FN_GROUPS, FN_GC, FN_GC), FN_GC ** -0.5),
        'w_out': nrm((L, D_MIX, D_MODEL), D_MIX ** -0.5),
        'norm2_w': gain((L, D_MODEL)),
        'peer_wq': nrm((L, D_MODEL, PEER_HEADS * PEER_QDIM), D_MODEL ** -0.5),
        'peer_k1': nrm((L, PEER_NKEYS, PEER_HALF), PEER_HALF ** -0.5),
        'peer_k2': nrm((L, PEER_NKEYS, PEER_HALF), PEER_HALF ** -0.5),
        'peer_u': nrm((L, PEER_N, D_MODEL), D_MODEL ** -0.5),
        'peer_v': nrm((L, PEER_N, D_MODEL), PEER_TOPK ** -0.5),
        'final_norm_w': gain((D_MODEL,)),
    }


def reference(x, norm1_w, w_in, gdn_conv_w, gdn_a_log, gdn_dt_bias, gdn_norm_w, hy_conv_w, hy_conv_b,
              hy_w1, hy_b1, hy_w2, hy_b2, hy_w3, hy_b3, hy_w4, hy_b4, hy_freq, hy_skip, fnet_w, w_out,
              norm2_w, peer_wq, peer_k1, peer_k2, peer_u, peer_v, final_norm_w):
    for l in range(DEPTH):
        h = rms_norm(x, norm1_w[l])
        p = jnp.einsum('bsd,dc->bsc', h, w_in[l])
        y_a = gdn_mixer(p, gdn_conv_w[l], gdn_a_log[l], gdn_dt_bias[l], gdn_norm_w[l])
        y_b = hyena_mixer(p, hy_conv_w[l], hy_conv_b[l], hy_w1[l], hy_b1[l], hy_w2[l], hy_b2[l],
                          hy_w3[l], hy_b3[l], hy_w4[l], hy_b4[l], hy_freq[l], hy_skip[l])
        y_c = fnet_mixer(p, fnet_w[l])
        mix = jnp.concatenate([y_a, y_b, y_c], axis=-1)
        x = x + jnp.einsum('bsc,cd->bsd', mix, w_out[l]).astype(x.dtype)
        h = rms_norm(x, norm2_w[l])
        x = x + peer_ffn(h, peer_wq[l], peer_k1[l], peer_k2[l], peer_u[l], peer_v[l]).astype(x.dtype)
    return rms_norm(x, final_norm_w)
```

```python
import numpy as np
from contextlib import ExitStack
import ml_dtypes
import concourse.bass as bass
import concourse.mybir as mybir
from concourse.bass_utils import run_bass_kernel_spmd

F32 = mybir.dt.float32
BF16 = mybir.dt.bfloat16
I32 = mybir.dt.int32
U32 = mybir.dt.uint32
AF = mybir.ActivationFunctionType
ALU = mybir.AluOpType
AX = mybir.AxisListType

ENGS = ("pe", "act", "dve", "pool", "sp")
NDMASEM = 40


class Res:
    __slots__ = ("w", "r", "name", "x")

    def __init__(self, name="", x=False):
        self.w = None
        self.r = []
        self.name = name
        self.x = x


class Op:
    __slots__ = ("eng", "fn", "deps", "sig", "sem", "val", "dma", "pos", "gen")


class Prog:
    def __init__(self, nc):
        self.nc = nc
        self.engs = {"pe": nc.tensor, "act": nc.scalar, "dve": nc.vector, "pool": nc.gpsimd, "sp": nc.sync}
        self.esem = {}
        self.dsem = []
        self.stream = {e: [] for e in ENGS}
        self.sigcount = {(e, i): 0 for e in ENGS for i in range(8)}
        self.dma_rr = 0
        self.dsem_last = [None] * NDMASEM
        self.dsem_val = [0] * NDMASEM
        self.seen = {e: {} for e in ENGS}
        self.pending_dma = []
        self.emitted = {e: 0 for e in ENGS}
        self.nops = 0
        self.gen = 0

    def alloc_sems(self, stack):
        self.nsets = 8
        for e in ENGS:
            if e == "sp":
                continue
            self.esem[e] = [stack.enter_context(self.nc.semaphore("es_%s%d" % (e, i))) for i in range(self.nsets)]
        for i in range(NDMASEM):
            self.dsem.append(stack.enter_context(self.nc.semaphore("ds%d" % i)))


    def op(self, eng, fn, reads=(), writes=(), dma=False):
        o = Op()
        o.eng = eng
        o.fn = fn
        o.dma = dma
        o.sig = False
        o.sem = None
        o.val = None
        o.gen = self.gen
        xr = [r for r in reads if r.x]
        if xr:
            reads = [r for r in reads if not r.x]
            writes = list(writes) + xr
        deps = []
        for r in reads:
            if r.w is not None:
                deps.append(r.w)
        for w in writes:
            if w.w is not None:
                deps.append(w.w)
            deps.extend(w.r)
        rawids = set(id(r.w) for r in reads if r.w is not None)
        need_same = any((not d.dma) and d.eng == eng and id(d) in rawids and d.gen >= self.gen for d in deps)
        latest = {}
        for d in deps:
            if d.dma or d.gen < self.gen:
                continue
            if d.eng not in latest or d.pos > latest[d.eng].pos:
                latest[d.eng] = d
        deps = [d for d in deps if d.dma or (d.gen >= self.gen and latest[d.eng] is d)]
        dd = []
        sid = set()
        for d in deps:
            if id(d) in sid or d.gen < self.gen:
                continue
            sid.add(id(d))
            if (not d.dma) and d.eng == eng and not dma:
                if not need_same or eng == "pe":
                    continue
            dd.append(d)
        if dma:
            k = self.dma_rr % NDMASEM
            self.dma_rr += 1
            prev = self.dsem_last[k]
            if prev is not None and prev.gen == self.gen:
                dd.append(prev)
            self.dsem_last[k] = o
            self.dsem_val[k] += 16
            o.sem = k
            o.val = self.dsem_val[k]
            o.sig = True
            self.pending_dma.append(o)
        o.deps = dd
        for d in dd:
            d.sig = True
        for r in reads:
            r.r.append(o)
        for w in writes:
            w.w = o
            w.r = []
        o.pos = len(self.stream[eng])
        self.stream[eng].append(o)
        self.nops += 1
        return o

    def barrier(self):
        lasts = []
        for e in ENGS:
            s = self.stream[e]
            if s:
                for o in reversed(s):
                    if o.gen < self.gen:
                        break
                    if not o.dma and o.fn is not None:
                        lasts.append(o)
                        break
        pend = list(self.pending_dma)
        self.pending_dma = []
        for e in ENGS:
            o = Op()
            o.eng = e
            o.fn = None
            o.dma = False
            o.sig = False
            o.sem = None
            o.val = None
            o.deps = [d for d in lasts if d.eng != e] + pend
            for d in o.deps:
                d.sig = True
            o.gen = self.gen
            o.pos = len(self.stream[e])
            self.stream[e].append(o)
        self.gen += 1

    def emit(self):
        for e in ENGS:
            for o in self.stream[e][self.emitted[e]:]:
                if o.dma or o.fn is None:
                    continue
                if o.sig:
                    ks = (e, o.gen % self.nsets)
                    self.sigcount[ks] += 1
                    o.val = self.sigcount[ks]
        prog = self

        def run(e, eng):
            seen = prog.seen[e]
            for o in prog.stream[e][prog.emitted[e]:]:
                for d in o.deps:
                    if d.dma:
                        key = ("d", d.sem)
                        sem = prog.dsem[d.sem]
                    else:
                        key = (d.eng, d.gen % prog.nsets)
                        sem = prog.esem[d.eng][d.gen % prog.nsets]
                    assert d.val is not None, (d.eng, d.dma)
                    if seen.get(key, 0) >= d.val:
                        continue
                    seen[key] = d.val
                    eng.wait_ge(sem, d.val)
                if o.fn is None:
                    continue
                ins = o.fn(eng)
                if o.dma:
                    ins.then_inc(prog.dsem[o.sem], 16)
                elif o.sig:
                    ins.then_inc(prog.esem[e][o.gen % prog.nsets], 1)
            prog.emitted[e] = len(prog.stream[e])

        with self.nc.Block() as block:
            @block.tensor
            def _(eng):
                run("pe", eng)

            @block.scalar
            def _(eng):
                run("act", eng)

            @block.vector
            def _(eng):
                run("dve", eng)

            @block.gpsimd
            def _(eng):
                run("pool", eng)

            @block.sync
            def _(eng):
                run("sp", eng)


D = 2048
S = 4096
NL = 2
EPS = 1e-6
KD = 16
TB = 512
NTB = S // TB
IN_COLS = 6176
OFF_Z, OFF_BA, OFF_HY, OFF_FN = 3072, 4096, 4128, 5664
NMAIN = 48
PC_Q, PC_K, PC_V, PC_Z, PC_HY, PC_FN = 0, 8, 16, 24, 32, 44


class T:
    def __init__(self, t, name):
        self.t = t
        self.r = Res(name)


class K:
    def __init__(self, dbg=()):
        self.nc = bass.Bass("TRN2", target_bir_lowering=False)
        self.P = Prog(self.nc)
        self.dbg = set(dbg)
        self.dram = {}

    def din(self, name, shape, dt=F32):
        self.dram[name] = self.nc.dram_tensor(name, list(shape), dt, kind="ExternalInput").ap()
        return self.dram[name]

    def dscr(self, name, shape, dt=F32, out=False):
        kind = "ExternalOutput" if (out or name in self.dbg) else None
        if kind:
            self.dram[name] = self.nc.dram_tensor(name, list(shape), dt, kind=kind).ap()
        else:
            self.dram[name] = self.nc.dram_tensor(name, list(shape), dt).ap()
        return self.dram[name]

    def sb(self, st, name, shape, dt=F32):
        self.ncnt = getattr(self, "ncnt", 0) + 1
        name = "%s_%d" % (name, self.ncnt)
        return T(st.enter_context(self.nc.sbuf_tensor(name, list(shape), dt)), name)

    def dma(self, eng, out, in_, reads=(), writes=()):
        w = list(writes) if writes else [Res("dmaout")]
        return self.P.op(eng, lambda e: e.dma_start(out=out, in_=in_), reads=reads, writes=w, dma=True)


def build(dbg=(), stages=("A", "G", "H", "F", "E", "P", "N"), nl=NL, gsteps=32, g3=True, g1=True, gstop=99):
    k = K(dbg)
    nc, P = k.nc, k.P
    xT = k.din("xT", [D, S])
    n1 = k.din("n1", [NL, 128, KD])
    n2 = k.din("n2", [NL, 128, KD])
    nf = k.din("nf", [128, KD])
    w_main = k.din("w_main", [NL, NMAIN, 128, KD * 128])
    w_ba = k.din("w_ba", [NL, 128, KD * 32])
    if "E" in stages:
        w_out = k.din("w_out", [NL, KD, 128, KD * 128])
    if "F" in stages:
        fn_w = k.din("fn_w", [NL, 128, 4 * 128])
        dftc = k.din("dftc", [2, 128, 128])
        fcs = k.din("fcs", [2, 16, 128, 32 * 256], BF16)
    if "G" in stages:
        g_cw = k.din("g_cw", [NL, 128, 72])
        g_par = k.din("g_par", [NL, 128, 1024])
        g_nw = k.din("g_nw", [NL, 128, 128])
        gmask = k.din("gmask", [6, 128, 128])
        gsel = k.din("gsel", [8, 1024])
        gq = k.dscr("gq", [3072, S])
        go = k.dscr("go", [2, S, 1024])
    if "P" in stages:
        p_wq = k.din("p_wq", [NL, KD, 128, KD * 128])
        p_kT = k.din("p_kT", [NL, 2, 128, 128])
        p_iota = k.din("p_iota", [128, 128])
        p_uT = k.din("p_uT", [NL, 128, 128, KD * 128])
        p_v = k.din("p_v", [NL, KD, 8, 128, 16 * 128])
    if "H" in stages:
        h_z = k.din("h_z", [33, S])
        h_w1 = k.din("h_w1", [NL, 33, 64])
        h_w23 = k.din("h_w23", [NL, 2, 64, 64])
        h_w4b = k.din("h_w4b", [NL, 65, 1024])
        h_bf = k.din("h_bf", [NL, 64, 4])
        h_win = k.din("h_win", [32, 128, 512])
        h_tf = k.din("h_tf", [2, 16, 128, 32 * 256], BF16)
        h_ti = k.din("h_ti", [2, 16, 128, 32 * 256], BF16)
        h_cw = k.din("h_cw", [NL, 128, 48])
        h_skip = k.din("h_skip", [NL, 128, 4])
        hspec = k.dscr("hspec", [2, S, 512])
        hvg = k.dscr("hvg", [512, S])
        hx1 = k.dscr("hx1", [512, S])
    pT = k.dscr("pT", [NMAIN * 128, S])
    baT = k.dscr("baT", [32, S])
    batok = k.dscr("batok", [128, 32 * 32])
    mixT = k.dscr("mixT", [D, S], BF16)
    x1T = k.dscr("x1T", [D, S])
    x2T = k.dscr("x2T", [D, S])
    yT = k.dscr("yT", [D, S], out=True)

    with ExitStack() as gst:
        P.alloc_sems(gst)
        PS = [T(gst.enter_context(nc.psum_tensor("psb%d" % i, [128, 512], F32)), "psb%d" % i) for i in range(8)]
        for t_ in PS:
            t_.r.x = True
        ones = k.sb(gst, "ones", [128, 128])
        P.op("pool", lambda e: e.memset(ones.t[:], 1.0), writes=[ones.r])
        ident = k.sb(gst, "ident", [128, 128])
        P.op("pool", lambda e: e.memset(ident.t[:], 1.0), writes=[ident.r])
        P.op("pool", lambda e: e.affine_select(out=ident.t[:], in_=ident.t[:], pattern=[[-1, 128]], compare_op=ALU.is_equal,
                                               fill=0.0, base=0, channel_multiplier=1), reads=[ident.r], writes=[ident.r])

        def rmsnorm(xt, hT, nw, sq, rstd, psb, N=TB):
            for kk in range(KD):
                j = kk % 2
                P.op("act", lambda e, kk=kk, j=j: e.activation(out=sq[j].t[:, :N], in_=xt.t[:, kk, :N], func=AF.Square),
                     reads=[xt.r], writes=[sq[j].r])
                P.op("pe", lambda e, kk=kk, j=j: e.matmul(psb.t[:, :N], lhsT=ones.t[:], rhs=sq[j].t[:, :N], start=(kk == 0), stop=(kk == KD - 1)),
                     reads=[sq[j].r, ones.r], writes=[psb.r])
            P.op("act", lambda e: e.activation(out=rstd.t[:, :N], in_=psb.t[:, :N], func=AF.Sqrt, bias=EPS, scale=1.0 / D),
                 reads=[psb.r], writes=[rstd.r])
            P.op("dve", lambda e: e.reciprocal(out=rstd.t[:, :N], in_=rstd.t[:, :N]), reads=[rstd.r], writes=[rstd.r])
            for kk in range(KD):
                P.op("dve", lambda e, kk=kk: e.scalar_tensor_tensor(out=hT.t[:, kk, :N], in0=xt.t[:, kk, :N], scalar=nw.t[:, kk:kk + 1], in1=rstd.t[:, :N],
                                                                  op0=ALU.mult, op1=ALU.mult),
                     reads=[xt.r, nw.r, rstd.r], writes=[hT.r])

        cur = xT
        for l in range(nl):
            if "A" in stages:
                with ExitStack() as st:
                    xt = k.sb(st, "A_x", [128, KD, TB])
                    hT = k.sb(st, "A_h", [128, KD, TB], BF16)
                    sq = [k.sb(st, "A_sq%d" % i, [128, TB]) for i in range(2)]
                    rstd = k.sb(st, "A_rstd", [128, TB])
                    nw = k.sb(st, "A_nw", [128, KD])
                    wt = [k.sb(st, "A_w%d" % i, [128, KD, 128], BF16) for i in range(3)]
                    wba = k.sb(st, "A_wba", [128, KD, 32], BF16)
                    ot = [k.sb(st, "A_o%d" % i, [128, TB]) for i in range(3)]
                    obt = [k.sb(st, "A_ob%d" % i, [128, 32]) for i in range(2)]
                    k.dma("sp", nw.t[:], n1[l], writes=[nw.r])
                    k.dma("pool", wba.t[:].rearrange("p k c -> p (k c)"), w_ba[l], writes=[wba.r])
                    for tb in range(NTB):
                        ts = slice(tb * TB, (tb + 1) * TB)
                        k.dma("sp", xt.t[:], cur.rearrange("(k p) t -> p k t", p=128)[:, :, ts], writes=[xt.r])
                        rmsnorm(xt, hT, nw, sq, rstd, PS[0])
                        for c in range(NMAIN):
                            w = wt[c % 3]
                            k.dma("pool", w.t[:].rearrange("p k c -> p (k c)"), w_main[l, c], writes=[w.r])
                            pb = PS[1 + c % 4]
                            for kk in range(KD):
                                P.op("pe", lambda e, kk=kk, w=w, pb=pb: e.matmul(pb.t[:], lhsT=w.t[:, kk, :], rhs=hT.t[:, kk, :], start=(kk == 0), stop=(kk == KD - 1)),
                                     reads=[w.r, hT.r], writes=[pb.r])
                            o = ot[c % 3]
                            if c % 2 == 0:
                                P.op("act", lambda e, o=o, pb=pb: e.activation(out=o.t[:], in_=pb.t[:], func=AF.Copy), reads=[pb.r], writes=[o.r])
                            else:
                                P.op("dve", lambda e, o=o, pb=pb: e.tensor_copy(out=o.t[:], in_=pb.t[:]), reads=[pb.r], writes=[o.r])
                            k.dma("sp", pT[c * 128:(c + 1) * 128, ts], o.t[:], reads=[o.r])
                        pb = PS[5]
                        for kk in range(KD):
                            P.op("pe", lambda e, kk=kk, pb=pb: e.matmul(pb.t[0:32, :], lhsT=wba.t[:, kk, :], rhs=hT.t[:, kk, :], start=(kk == 0), stop=(kk == KD - 1)),
                                 reads=[wba.r, hT.r], writes=[pb.r])
                        o = ot[0]
                        P.op("act", lambda e, o=o, pb=pb: e.activation(out=o.t[0:32, :], in_=pb.t[0:32, :], func=AF.Copy), reads=[pb.r], writes=[o.r])
                        k.dma("sp", baT[:, ts], o.t[0:32, :], reads=[o.r])
                        for tt in range(TB // 128):
                            pb = PS[6 + tt % 2]
                            for kk in range(KD):
                                P.op("pe", lambda e, kk=kk, pb=pb, tt=tt: e.matmul(pb.t[:, 0:32], lhsT=hT.t[:, kk, tt * 128:(tt + 1) * 128], rhs=wba.t[:, kk, :], start=(kk == 0), stop=(kk == KD - 1)),
                                     reads=[wba.r, hT.r], writes=[pb.r])
                            ob = obt[tt % 2]
                            P.op("dve", lambda e, ob=ob, pb=pb: e.tensor_copy(out=ob.t[:], in_=pb.t[:, 0:32]), reads=[pb.r], writes=[ob.r])
                            k.dma("sp", batok[:, (tb * 4 + tt) * 32:(tb * 4 + tt + 1) * 32], ob.t[:], reads=[ob.r])
                    P.barrier()
                    P.emit()


            if "G" in stages:
                NEG = -1.0e30
                with ExitStack() as st:
                  if g1:
                      pin = [k.sb(st, "G1_in%d" % i, [128, S + 2]) for i in range(2)]
                      uu = [k.sb(st, "G1_u%d" % i, [128, S]) for i in range(2)]
                      sqt = [k.sb(st, "G1_sq%d" % i, [128, 512]) for i in range(2)]
                      rn = [k.sb(st, "G1_rn%d" % i, [128, 512]) for i in range(2)]
                      cw = k.sb(st, "G1_cw", [128, 24, 3])
                      k.dma("sp", cw.t[:].rearrange("p c t -> p (c t)"), g_cw[l], writes=[cw.r])
                      for i in range(2):
                          P.op("pool", lambda e, i=i: e.memset(pin[i].t[:, 0:1], 0.0), writes=[pin[i].r])
                          P.op("pool", lambda e, i=i: e.memset(pin[i].t[:, S + 1:S + 2], 0.0), writes=[pin[i].r])
                      for ch in range(24):
                          pi, u = pin[ch % 2], uu[ch % 2]
                          k.dma("sp", pi.t[:, 1:S + 1], pT[ch * 128:(ch + 1) * 128, :], writes=[pi.r])
                          P.op("dve", lambda e, pi=pi, u=u, ch=ch: e.tensor_scalar(out=u.t[:], in0=pi.t[:, 1:S + 1], scalar1=cw.t[:, ch, 1:2], scalar2=None, op0=ALU.mult), reads=[pi.r, cw.r], writes=[u.r])
                          P.op("dve", lambda e, pi=pi, u=u, ch=ch: e.scalar_tensor_tensor(out=u.t[:], in0=pi.t[:, 0:S], scalar=cw.t[:, ch, 0:1], in1=u.t[:], op0=ALU.mult, op1=ALU.add), reads=[pi.r, cw.r, u.r], writes=[u.r])
                          P.op("dve", lambda e, pi=pi, u=u, ch=ch: e.scalar_tensor_tensor(out=u.t[:], in0=pi.t[:, 2:S + 2], scalar=cw.t[:, ch, 2:3], in1=u.t[:], op0=ALU.mult, op1=ALU.add), reads=[pi.r, cw.r, u.r], writes=[u.r])
                          P.op("act", lambda e, u=u: e.activation(out=u.t[:], in_=u.t[:], func=AF.Silu), reads=[u.r], writes=[u.r])
                          if ch < 16:
                              for b8 in range(8):
                                  sl = slice(b8 * 512, (b8 + 1) * 512)
                                  sq_, rn_ = sqt[b8 % 2], rn[b8 % 2]
                                  pb = PS[b8 % 8]
                                  P.op("act", lambda e, u=u, sq_=sq_, sl=sl: e.activation(out=sq_.t[:], in_=u.t[:, sl], func=AF.Square), reads=[u.r], writes=[sq_.r])
                                  P.op("pe", lambda e, pb=pb, sq_=sq_: e.matmul(pb.t[:], lhsT=ones.t[:], rhs=sq_.t[:], start=True, stop=True), reads=[ones.r, sq_.r], writes=[pb.r])
                                  P.op("act", lambda e, pb=pb, rn_=rn_: e.activation(out=rn_.t[:], in_=pb.t[:], func=AF.Sqrt, bias=EPS, scale=1.0), reads=[pb.r], writes=[rn_.r])
                                  P.op("dve", lambda e, rn_=rn_: e.reciprocal(out=rn_.t[:], in_=rn_.t[:]), reads=[rn_.r], writes=[rn_.r])
                                  P.op("dve", lambda e, u=u, rn_=rn_, sl=sl, ch=ch: e.scalar_tensor_tensor(out=u.t[:, sl], in0=u.t[:, sl], scalar=(128.0 ** -0.5 if ch < 8 else 1.0), in1=rn_.t[:], op0=ALU.mult, op1=ALU.mult), reads=[u.r, rn_.r], writes=[u.r])
                          k.dma("sp", gq[ch * 128:(ch + 1) * 128, :], u.t[:], reads=[u.r])
                      P.barrier()
                      P.emit()
                with ExitStack() as st:
                  if gsteps >= 0:
                      maskS = [k.sb(st, "G_mS%d" % i, [128, 128]) for i in range(2)]
                      maskI = [k.sb(st, "G_mI%d" % i, [128, 128]) for i in range(2)]
                      tri = [k.sb(st, "G_tri%d" % i, [128, 128]) for i in range(2)]
                      sel = k.sb(st, "G_sel", [8, 8, 128])
                      for i in range(2):
                          k.dma("sp", maskS[i].t[:], gmask[i], writes=[maskS[i].r])
                          k.dma("sp", maskI[i].t[:], gmask[2 + i], writes=[maskI[i].r])
                          k.dma("sp", tri[i].t[:], gmask[4 + i], writes=[tri[i].r])
                      k.dma("sp", sel.t[:].rearrange("p h m -> p (h m)"), gsel, writes=[sel.r])
                      if gstop <= 1:
                          P.barrier(); P.emit(); return k
                      bt = k.sb(st, "G_bt", [128, 32, 32])
                      par = k.sb(st, "G_par", [128, 2, 512])
                      beta = k.sb(st, "G_beta", [128, 2, 32, 8])
                      gt = k.sb(st, "G_g", [128, 2, 32, 8])
                      Gc = k.sb(st, "G_Gc", [128, 2, 32, 8])
                      nGc = k.sb(st, "G_nGc", [128, 2, 32, 8])
                      bexp = k.sb(st, "G_bexp", [128, 2, 32, 8])
                      kdec = k.sb(st, "G_kdec", [128, 2, 32, 8])
                      etot = k.sb(st, "G_etot", [128, 2, 32, 8])
                      k.dma("sp", bt.t[:].rearrange("p c j -> p (c j)"), batok, writes=[bt.r])
                      k.dma("sp", par.t[:].rearrange("p a b -> p (a b)"), g_par[l], writes=[par.r])
                      fl = lambda t_: t_.t[:].rearrange("p d c h -> p (d c h)")
                      P.op("act", lambda e: e.activation(out=par.t[:, 0, :], in_=par.t[:, 0, :], func=AF.Exp), reads=[par.r], writes=[par.r])
                      for d in range(2):
                          P.op("act", lambda e, d=d: e.activation(out=beta.t[:, d], in_=bt.t[:, :, d * 8:(d + 1) * 8], func=AF.Sigmoid), reads=[bt.r], writes=[beta.r])
                          P.op("dve", lambda e, d=d: e.tensor_copy(out=gt.t[:, d], in_=bt.t[:, :, 16 + d * 8:16 + (d + 1) * 8]), reads=[bt.r], writes=[gt.r])
                      P.op("dve", lambda e: e.tensor_tensor(out=fl(gt), in0=fl(gt), in1=par.t[:, 1, :], op=ALU.add), reads=[gt.r, par.r], writes=[gt.r])
                      P.op("act", lambda e: e.activation(out=fl(gt), in_=fl(gt), func=AF.Exp), reads=[gt.r], writes=[gt.r])
                      P.op("act", lambda e: e.activation(out=fl(gt), in_=fl(gt), func=AF.Ln, bias=1.0), reads=[gt.r], writes=[gt.r])
                      P.op("dve", lambda e: e.tensor_tensor(out=fl(gt), in0=fl(gt), in1=par.t[:, 0, :], op=ALU.mult), reads=[gt.r, par.r], writes=[gt.r])
                      P.op("dve", lambda e: e.tensor_scalar(out=fl(gt), in0=fl(gt), scalar1=-1.0, scalar2=None, op0=ALU.mult), reads=[gt.r], writes=[gt.r])
                      if gstop <= 2:
                          P.barrier(); P.emit(); return k
                      for d in range(2):
                          P.op("pe", lambda e, d=d: e.matmul(PS[d].t[:, 0:256], lhsT=tri[d].t[:], rhs=gt.t[:, d].rearrange("p c h -> p (c h)"), start=True, stop=True), reads=[tri[d].r, gt.r], writes=[PS[d].r])
                          P.op("pe", lambda e, d=d: e.matmul(PS[2 + d].t[:, 0:256], lhsT=ones.t[:], rhs=gt.t[:, d].rearrange("p c h -> p (c h)"), start=True, stop=True), reads=[ones.r, gt.r], writes=[PS[2 + d].r])
                          P.op("dve", lambda e, d=d: e.tensor_copy(out=Gc.t[:, d].rearrange("p c h -> p (c h)"), in_=PS[d].t[:, 0:256]), reads=[PS[d].r], writes=[Gc.r])
                          P.op("act", lambda e, d=d: e.activation(out=etot.t[:, d].rearrange("p c h -> p (c h)"), in_=PS[2 + d].t[:, 0:256], func=AF.Exp), reads=[PS[2 + d].r], writes=[etot.r])
                          P.op("dve", lambda e, d=d: e.tensor_tensor(out=kdec.t[:, d].rearrange("p c h -> p (c h)"), in0=PS[2 + d].t[:, 0:256], in1=Gc.t[:, d].rearrange("p c h -> p (c h)"), op=ALU.subtract), reads=[PS[2 + d].r, Gc.r], writes=[kdec.r])
                      P.op("act", lambda e: e.activation(out=fl(kdec), in_=fl(kdec), func=AF.Exp), reads=[kdec.r], writes=[kdec.r])
                      P.op("act", lambda e: e.activation(out=fl(bexp), in_=fl(Gc), func=AF.Exp), reads=[Gc.r], writes=[bexp.r])
                      P.op("dve", lambda e: e.tensor_tensor(out=fl(bexp), in0=fl(bexp), in1=fl(beta), op=ALU.mult), reads=[bexp.r, beta.r], writes=[bexp.r])
                      P.op("dve", lambda e: e.tensor_scalar(out=fl(nGc), in0=fl(Gc), scalar1=-1.0, scalar2=None, op0=ALU.mult), reads=[Gc.r], writes=[nGc.r])
                      if gstop <= 3:
                          P.barrier(); P.emit(); return k
                      P.barrier()
                      P.emit()
                      Sst = [[k.sb(st, "G_S%d_%d" % (d, h), [128, 128]) for h in range(8)] for d in range(2)]
                      for d in range(2):
                          for h in range(8):
                              P.op("pool", lambda e, d=d, h=h: e.memset(Sst[d][h].t[:], 0.0), writes=[Sst[d][h].r])
                      NPB = 8
                      names = ["Dm", "A", "QKDT", "MT", "Pa", "Pb", "PTa", "PTb", "Rv", "Rk", "u", "wT", "vn", "qg", "kd"]
                      pbuf = [{n: k.sb(st, "G_%s%d" % (n, i), [128, 128]) for n in names} for i in range(NPB)]
                      qkv = [[k.sb(st, "G_qkv%d_%d" % (d, t), [128, 8, 128]) for t in range(3)] for d in range(2)]
                      ktok = [k.sb(st, "G_ktok%d" % d, [128, 8, 128]) for d in range(2)]
                      vtok = [k.sb(st, "G_vtok%d" % d, [128, 8, 128]) for d in range(2)]
                      grow = [k.sb(st, "G_grow%d" % d, [8, 128]) for d in range(2)]
                      osb = [k.sb(st, "G_o%d" % d, [128, 8, 128]) for d in range(2)]
                      sc_ = [0]

                      def slot():
                          i = sc_[0] % 8
                          sc_[0] += 1
                          return PS[i].t[:, 0:128], PS[i].r

                      def mm(out, outr, lhsT, rhs, reads):
                          P.op("pe", lambda e: e.matmul(out, lhsT=lhsT, rhs=rhs, start=True, stop=True), reads=reads, writes=[outr])

                      ecnt = [0]

                      def evac(dst, src, srcr):
                          ecnt[0] += 1
                          if ecnt[0] % 2 == 0:
                              P.op("act", lambda e: e.activation(out=dst.t[:], in_=src, func=AF.Copy), reads=[srcr], writes=[dst.r])
                          else:
                              P.op("dve", lambda e: e.tensor_copy(out=dst.t[:], in_=src), reads=[srcr], writes=[dst.r])

                      def gstep(step, d):
                          c = step if d == 0 else 31 - step
                          cs = slice(c * 128, (c + 1) * 128)
                          q3 = qkv[d]
                          for t in range(3):
                              k.dma("sp", q3[t].t[:], gq[t * 1024:(t + 1) * 1024, cs].rearrange("(h p) s -> p h s", p=128), writes=[q3[t].r])
                          qT, kT, vT = q3
                          for h in range(8):
                              for (src, dst) in ((kT, ktok[d]), (vT, vtok[d])):
                                  ps_, pr_ = slot()
                                  P.op("pe", lambda e, ps_=ps_, src=src, h=h: e.transpose(ps_, src.t[:, h, :], ident.t[:]), reads=[src.r, ident.r], writes=[pr_])
                                  ecnt[0] += 1
                                  if ecnt[0] % 2 == 0:
                                      P.op("act", lambda e, ps_=ps_, dst=dst, h=h: e.activation(out=dst.t[:, h, :], in_=ps_, func=AF.Copy), reads=[pr_], writes=[dst.r])
                                  else:
                                      P.op("dve", lambda e, ps_=ps_, dst=dst, h=h: e.tensor_copy(out=dst.t[:, h, :], in_=ps_), reads=[pr_], writes=[dst.r])
                          ps_, pr_ = slot()
                          P.op("pe", lambda e, ps_=ps_, d=d, c=c: e.transpose(ps_[0:8, :], Gc.t[:, d, c, :], ident.t[:]), reads=[Gc.r, ident.r], writes=[pr_])
                          P.op("dve", lambda e, ps_=ps_, d=d: e.tensor_copy(out=grow[d].t[:], in_=ps_[0:8, :]), reads=[pr_], writes=[grow[d].r])
                          col = lambda tl, h: tl.t[:, d, c, h:h + 1]
                          B = pbuf
                          gb = []
                          for h in range(8):
                              ps_, pr_ = slot()
                              mm(ps_, pr_, sel.t[:, h, :], grow[d].t[:], [sel.r, grow[d].r])
                              gb.append((ps_, pr_))
                          for h in range(8):
                              ps_, pr_ = gb[h]
                              b = B[h]
                              P.op("dve", lambda e, ps_=ps_, b=b, d=d: e.scalar_tensor_tensor(out=b["Dm"].t[:], in0=ps_, scalar=-1.0, in1=maskS[d].t[:], op0=ALU.mult, op1=ALU.add), reads=[pr_, maskS[d].r], writes=[b["Dm"].r])
                              P.op("act", lambda e, b=b, h=h, d=d, c=c: e.activation(out=b["Dm"].t[:], in_=b["Dm"].t[:], func=AF.Exp, bias=Gc.t[:, d, c, h:h + 1]), reads=[b["Dm"].r, Gc.r], writes=[b["Dm"].r])
                              P.op("dve", lambda e, ps_=ps_, b=b, d=d: e.tensor_tensor(out=b["QKDT"].t[:], in0=ps_, in1=maskI[d].t[:], op=ALU.add), reads=[pr_, maskI[d].r], writes=[b["QKDT"].r])
                              P.op("act", lambda e, b=b, h=h, d=d, c=c: e.activation(out=b["QKDT"].t[:], in_=b["QKDT"].t[:], func=AF.Exp, bias=nGc.t[:, d, c, h:h + 1]), reads=[b["QKDT"].r, nGc.r], writes=[b["QKDT"].r])
                              P.op("act", lambda e, ps_=ps_, b=b: e.activation(out=b["qg"].t[:], in_=ps_, func=AF.Exp), reads=[pr_], writes=[b["qg"].r])
                              P.op("dve", lambda e, b=b, h=h: e.tensor_tensor(out=b["qg"].t[:], in0=b["qg"].t[:], in1=qT.t[:, h, :], op=ALU.mult), reads=[b["qg"].r, qT.r], writes=[b["qg"].r])
                          for h in range(8):
                              b = B[h]
                              ps_, pr_ = slot()
                              mm(ps_, pr_, kT.t[:, h, :], kT.t[:, h, :], [kT.r])
                              P.op("dve", lambda e, ps_=ps_, b=b, h=h, d=d, c=c: e.scalar_tensor_tensor(out=b["A"].t[:], in0=ps_, scalar=beta.t[:, d, c, h:h + 1], in1=b["Dm"].t[:], op0=ALU.mult, op1=ALU.mult), reads=[pr_, beta.r, b["Dm"].r], writes=[b["A"].r])
                              ps2, pr2 = slot()
                              mm(ps2, pr2, kT.t[:, h, :], qT.t[:, h, :], [kT.r, qT.r])
                              P.op("dve", lambda e, ps2=ps2, b=b: e.tensor_tensor(out=b["QKDT"].t[:], in0=ps2, in1=b["QKDT"].t[:], op=ALU.mult), reads=[pr2, b["QKDT"].r], writes=[b["QKDT"].r])
                              P.op("pool", lambda e, b=b, h=h, d=d, c=c: e.tensor_scalar(out=b["Rv"].t[:], in0=vtok[d].t[:, h, :], scalar1=beta.t[:, d, c, h:h + 1], scalar2=None, op0=ALU.mult), reads=[vtok[d].r, beta.r], writes=[b["Rv"].r])
                              P.op("pool", lambda e, b=b, h=h, d=d, c=c: e.tensor_scalar(out=b["Rk"].t[:], in0=ktok[d].t[:, h, :], scalar1=bexp.t[:, d, c, h:h + 1], scalar2=None, op0=ALU.mult), reads=[ktok[d].r, bexp.r], writes=[b["Rk"].r])
                              P.op("pool", lambda e, b=b, h=h, d=d, c=c: e.tensor_scalar(out=b["kd"].t[:], in0=ktok[d].t[:, h, :], scalar1=kdec.t[:, d, c, h:h + 1], scalar2=None, op0=ALU.mult), reads=[ktok[d].r, kdec.r], writes=[b["kd"].r])
                          for h in range(8):
                              b = B[h]
                              ps_, pr_ = slot()
                              P.op("pe", lambda e, ps_=ps_, b=b: e.transpose(ps_, b["A"].t[:], ident.t[:]), reads=[b["A"].r, ident.r], writes=[pr_])
                              P.op("dve", lambda e, ps_=ps_, b=b: e.scalar_tensor_tensor(out=b["MT"].t[:], in0=ps_, scalar=-1.0, in1=ident.t[:], op0=ALU.mult, op1=ALU.add), reads=[pr_, ident.r], writes=[b["MT"].r])
                              P.op("act", lambda e, ps_=ps_, b=b: e.activation(out=b["PTa"].t[:], in_=ps_, func=AF.Copy), reads=[pr_], writes=[b["PTa"].r])
                          Pc = ["A"] * 8
                          PTc = ["PTa"] * 8
                          for kk in range(1, 7):
                              for h in range(8):
                                  b = B[h]
                                  Pn = "Pa" if Pc[h] != "Pa" else "Pb"
                                  ps_, pr_ = slot()
                                  mm(ps_, pr_, b[PTc[h]].t[:], b[Pc[h]].t[:], [b[PTc[h]].r, b[Pc[h]].r])
                                  if kk < 6:
                                      PTn = "PTb" if PTc[h] == "PTa" else "PTa"
                                      ps2, pr2 = slot()
                                      mm(ps2, pr2, b[Pc[h]].t[:], b[PTc[h]].t[:], [b[PTc[h]].r, b[Pc[h]].r])
                                  evac(b[Pn], ps_, pr_)
                                  if kk < 6:
                                      evac(b[PTn], ps2, pr2)
                                      PTc[h] = PTn
                                  Pc[h] = Pn
                                  ps3, pr3 = slot()
                                  mm(ps3, pr3, b[Pn].t[:], b["MT"].t[:], [b[Pn].r, b["MT"].r])
                                  P.op("dve", lambda e, ps3=ps3, b=b: e.tensor_tensor(out=b["MT"].t[:], in0=ps3, in1=b["MT"].t[:], op=ALU.add), reads=[pr3, b["MT"].r], writes=[b["MT"].r])
                          for h in range(8):
                              b = B[h]
                              ps_, pr_ = slot()
                              mm(ps_, pr_, b["MT"].t[:], b["Rv"].t[:], [b["MT"].r, b["Rv"].r])
                              evac(b["u"], ps_, pr_)
                              ps2, pr2 = slot()
                              mm(ps2, pr2, b["Rk"].t[:], b["MT"].t[:], [b["MT"].r, b["Rk"].r])
                              evac(b["wT"], ps2, pr2)
                          for h in range(8):
                              b = B[h]
                              Sh = Sst[d][h]
                              ps_, pr_ = slot()
                              mm(ps_, pr_, b["wT"].t[:], Sh.t[:], [b["wT"].r, Sh.r])
                              P.op("dve", lambda e, ps_=ps_, b=b: e.tensor_tensor(out=b["vn"].t[:], in0=b["u"].t[:], in1=ps_, op=ALU.subtract), reads=[pr_, b["u"].r], writes=[b["vn"].r])
                          for h in range(8):
                              b = B[h]
                              Sh = Sst[d][h]
                              ps_, pr_ = slot()

                              def two(e, ps_=ps_, b=b, Sh=Sh):
                                  e.matmul(ps_, lhsT=b["qg"].t[:], rhs=Sh.t[:], start=True, stop=False)
                                  return e.matmul(ps_, lhsT=b["QKDT"].t[:], rhs=b["vn"].t[:], start=False, stop=True)
                              P.op("pe", two, reads=[b["qg"].r, Sh.r, b["QKDT"].r, b["vn"].r], writes=[pr_])
                              ecnt[0] += 1
                              if ecnt[0] % 2 == 0:
                                  P.op("act", lambda e, ps_=ps_, h=h, d=d: e.activation(out=osb[d].t[:, h, :], in_=ps_, func=AF.Copy), reads=[pr_], writes=[osb[d].r])
                              else:
                                  P.op("dve", lambda e, ps_=ps_, h=h, d=d: e.tensor_copy(out=osb[d].t[:, h, :], in_=ps_), reads=[pr_], writes=[osb[d].r])
                              ps2, pr2 = slot()
                              mm(ps2, pr2, b["kd"].t[:], b["vn"].t[:], [b["kd"].r, b["vn"].r])
                              P.op("dve", lambda e, ps2=ps2, Sh=Sh, h=h, d=d, c=c: e.scalar_tensor_tensor(out=Sh.t[:], in0=Sh.t[:], scalar=etot.t[:, d, c, h:h + 1], in1=ps2, op0=ALU.mult, op1=ALU.add), reads=[pr2, Sh.r, etot.r], writes=[Sh.r])
                          k.dma("sp", go[d, cs, :], osb[d].t[:].rearrange("p h v -> p (h v)"), reads=[osb[d].r])

                      for step in range(gsteps):
                          for d in range(2):
                              gstep(step, d)
                          if step % 8 == 7 and step != gsteps - 1:
                              P.barrier()
                              P.emit()
                      P.barrier()
                      P.emit()
                with ExitStack() as st:
                  if g3:
                      of = [k.sb(st, "G3_of%d" % i, [128, 8, 128]) for i in range(2)]
                      ob_ = [k.sb(st, "G3_ob%d" % i, [128, 8, 128]) for i in range(2)]
                      sq3 = k.sb(st, "G3_sq", [128, 8, 128])
                      ss = [k.sb(st, "G3_ss%d" % i, [128, 8]) for i in range(2)]
                      zt = [k.sb(st, "G3_z%d" % i, [128, 8, 128]) for i in range(2)]
                      nwb = k.sb(st, "G3_nw", [128, 128])
                      yo = [k.sb(st, "G3_y%d" % i, [128, 8, 128], BF16) for i in range(2)]
                      k.dma("sp", nwb.t[:], g_nw[l], writes=[nwb.r])
                      for c in range(32):
                          cs = slice(c * 128, (c + 1) * 128)
                          o1, o2, z_, y_, s_ = of[c % 2], ob_[c % 2], zt[c % 2], yo[c % 2], ss[c % 2]
                          k.dma("sp", o1.t[:].rearrange("p h v -> p (h v)"), go[0, cs, :], writes=[o1.r])
                          k.dma("sp", o2.t[:].rearrange("p h v -> p (h v)"), go[1, cs, :], writes=[o2.r])
                          k.dma("sp", z_.t[:], pT[PC_Z * 128:(PC_Z + 8) * 128, cs].rearrange("(h p) s -> p h s", p=128), writes=[z_.r])
                          P.op("act", lambda e, z_=z_: e.activation(out=z_.t[:], in_=z_.t[:], func=AF.Silu), reads=[z_.r], writes=[z_.r])
                          P.op("dve", lambda e, o1=o1, o2=o2: e.tensor_tensor(out=o1.t[:], in0=o1.t[:], in1=o2.t[:], op=ALU.add), reads=[o1.r, o2.r], writes=[o1.r])
                          P.op("dve", lambda e, o1=o1: e.tensor_tensor(out=sq3.t[:], in0=o1.t[:], in1=o1.t[:], op=ALU.mult), reads=[o1.r], writes=[sq3.r])
                          P.op("dve", lambda e, s_=s_: e.tensor_reduce(out=s_.t[:], in_=sq3.t[:], axis=AX.X, op=ALU.add), reads=[sq3.r], writes=[s_.r])
                          P.op("act", lambda e, s_=s_: e.activation(out=s_.t[:], in_=s_.t[:], func=AF.Sqrt, bias=EPS, scale=1.0 / 128), reads=[s_.r], writes=[s_.r])
                          P.op("dve", lambda e, s_=s_: e.reciprocal(out=s_.t[:], in_=s_.t[:]), reads=[s_.r], writes=[s_.r])
                          for h in range(8):
                              P.op("dve", lambda e, o1=o1, s_=s_, h=h: e.scalar_tensor_tensor(out=o1.t[:, h, :], in0=o1.t[:, h, :], scalar=s_.t[:, h:h + 1], in1=nwb.t[:], op0=ALU.mult, op1=ALU.mult), reads=[o1.r, s_.r, nwb.r], writes=[o1.r])
                          for h in range(8):
                              pb = PS[(c * 2 + h // 4) % 8]
                              q = h % 4
                              P.op("pe", lambda e, pb=pb, q=q, o1=o1, h=h: e.transpose(pb.t[:, q * 128:(q + 1) * 128], o1.t[:, h, :], ident.t[:]), reads=[o1.r, ident.r], writes=[pb.r])
                              if q == 3:
                                  h0 = h - 3
                                  P.op("dve", lambda e, pb=pb, y_=y_, z_=z_, h0=h0: e.tensor_tensor(out=y_.t[:, h0:h0 + 4, :].rearrange("p h s -> p (h s)"), in0=pb.t[:], in1=z_.t[:, h0:h0 + 4, :].rearrange("p h s -> p (h s)"), op=ALU.mult), reads=[pb.r, z_.r], writes=[y_.r])
                          k.dma("sp", mixT[0:1024, cs].rearrange("(h p) s -> p h s", p=128), y_.t[:], reads=[y_.r])
                      P.barrier()
                      P.emit()


            if "H" in stages:
                with ExitStack() as st:
                    HP = k.sb(st, "H_HP", [128, 32, 512], BF16)
                    HM = k.sb(st, "H_HM", [128, 32, 512], BF16)
                    rnorm = k.sb(st, "H_rn", [128, 512])
                    with ExitStack() as st1:
                        hz = k.sb(st1, "H_z", [33, S])
                        hid = [k.sb(st1, "H_hid%d" % i, [65, S]) for i in range(3)]
                        w1 = k.sb(st1, "H_w1", [33, 64])
                        w23 = k.sb(st1, "H_w23", [64, 2, 64])
                        w4b = k.sb(st1, "H_w4", [65, 1024])
                        bf_ = k.sb(st1, "H_bf", [64, 4])
                        fb = k.sb(st1, "H_fb", [64, 3])
                        ta = [k.sb(st1, "H_ta%d" % i, [64, 512]) for i in range(4)]
                        win = [k.sb(st1, "H_win%d" % i, [128, 512]) for i in range(2)]
                        hfb = [k.sb(st1, "H_hfb%d" % i, [128, 2, 512]) for i in range(2)]
                        ab = [k.sb(st1, "H_ab%d" % i, [128, 2, 512]) for i in range(2)]
                        k.dma("sp", hz.t[:], h_z, writes=[hz.r])
                        k.dma("sp", w1.t[:], h_w1[l], writes=[w1.r])
                        k.dma("sp", w23.t[:], h_w23[l].rearrange("a k j -> k a j"), writes=[w23.r])
                        k.dma("sp", w4b.t[:], h_w4b[l], writes=[w4b.r])
                        k.dma("sp", bf_.t[:], h_bf[l], writes=[bf_.r])
                        P.op("dve", lambda e: e.tensor_scalar(out=fb.t[:], in0=bf_.t[:, 0:3], scalar1=bf_.t[:, 3:4], scalar2=None, op0=ALU.mult), reads=[bf_.r], writes=[fb.r])
                        P.op("pool", lambda e: e.memset(hid[2].t[64:65, :], 1.0), writes=[hid[2].r])
                        for li in range(3):
                            src = hz if li == 0 else hid[li - 1]
                            kdim = 33 if li == 0 else 64
                            dst = hid[li]
                            for blk in range(8):
                                bs = slice(blk * 512, (blk + 1) * 512)
                                pb = PS[1 + (li * 8 + blk) % 7]
                                lw = w1.t[:, :] if li == 0 else w23.t[:, li - 1, :]
                                lwr = w1.r if li == 0 else w23.r
                                P.op("pe", lambda e, pb=pb, lw=lw, src=src, kdim=kdim, bs=bs: e.matmul(pb.t[0:64, :], lhsT=lw, rhs=src.t[0:kdim, bs], start=True, stop=True), reads=[lwr, src.r], writes=[pb.r])
                                a0, a1, a2, a3 = ta
                                P.op("dve", lambda e, pb=pb, li=li: e.tensor_scalar(out=a0.t[:], in0=pb.t[0:64, :], scalar1=bf_.t[:, 3:4], scalar2=fb.t[:, li:li + 1], op0=ALU.mult, op1=ALU.add), reads=[pb.r, bf_.r, fb.r], writes=[a0.r])
                                P.op("act", lambda e: e.activation(out=a1.t[:], in_=a0.t[:], func=AF.Sin, scale=0.5), reads=[a0.r], writes=[a1.r])
                                P.op("act", lambda e: e.activation(out=a2.t[:], in_=a0.t[:], func=AF.Sin, scale=0.25), reads=[a0.r], writes=[a2.r])
                                P.op("dve", lambda e: e.tensor_tensor(out=a2.t[:], in0=a2.t[:], in1=a2.t[:], op=ALU.mult), reads=[a2.r], writes=[a2.r])
                                P.op("dve", lambda e: e.tensor_scalar(out=a2.t[:], in0=a2.t[:], scalar1=-2.0, scalar2=1.0, op0=ALU.mult, op1=ALU.add), reads=[a2.r], writes=[a2.r])
                                P.op("dve", lambda e, dst=dst, bs=bs: e.scalar_tensor_tensor(out=dst.t[0:64, bs], in0=a1.t[:], scalar=2.0, in1=a2.t[:], op0=ALU.mult, op1=ALU.mult), reads=[a1.r, a2.r], writes=[dst.r])
                        for nc_ in range(32):
                            ns = slice(nc_ * 128, (nc_ + 1) * 128)
                            wn, hf, aa = win[nc_ % 2], hfb[nc_ % 2], ab[nc_ % 2]
                            k.dma("sp", wn.t[:], h_win[nc_], writes=[wn.r])
                            for half in range(2):
                                pb = PS[1 + (nc_ * 2 + half) % 7]
                                P.op("pe", lambda e, pb=pb, ns=ns, half=half: e.matmul(pb.t[:], lhsT=hid[2].t[0:65, ns], rhs=w4b.t[0:65, half * 512:(half + 1) * 512], start=True, stop=True), reads=[hid[2].r, w4b.r], writes=[pb.r])
                                P.op("dve", lambda e, pb=pb, hf=hf, wn=wn, half=half: e.tensor_tensor(out=hf.t[:, half, :], in0=pb.t[:], in1=wn.t[:], op=ALU.mult), reads=[pb.r, wn.r], writes=[hf.r])
                            if nc_ == 0:
                                P.op("dve", lambda e, hf=hf: e.memset(hf.t[0:1, 1, :], 0.0), reads=[hf.r], writes=[hf.r])
                            P.op("act", lambda e, hf=hf, aa=aa: e.activation(out=aa.t[:].rearrange("p a c -> p (a c)"), in_=hf.t[:].rearrange("p a c -> p (a c)"), func=AF.Abs), reads=[hf.r], writes=[aa.r])
                            P.op("dve", lambda e, aa=aa: e.tensor_tensor(out=aa.t[:, 0, :], in0=aa.t[:, 0, :], in1=aa.t[:, 1, :], op=ALU.add), reads=[aa.r], writes=[aa.r])
                            P.op("pe", lambda e, aa=aa, nc_=nc_: e.matmul(PS[0].t[:], lhsT=ones.t[:], rhs=aa.t[:, 0, :], start=(nc_ == 0), stop=(nc_ == 31)), reads=[ones.r, aa.r], writes=[PS[0].r])
                            P.op("pool", lambda e, hf=hf, nc_=nc_: e.tensor_tensor(out=HP.t[:, nc_, :], in0=hf.t[:, 0, :], in1=hf.t[:, 1, :], op=ALU.add), reads=[hf.r], writes=[HP.r])
                            P.op("pool", lambda e, hf=hf, nc_=nc_: e.tensor_tensor(out=HM.t[:, nc_, :], in0=hf.t[:, 0, :], in1=hf.t[:, 1, :], op=ALU.subtract), reads=[hf.r], writes=[HM.r])
                        P.op("dve", lambda e: e.reciprocal(out=rnorm.t[:], in_=PS[0].t[:]), reads=[PS[0].r], writes=[rnorm.r])
                        P.barrier()
                        P.emit()
                    with ExitStack() as st1:
                        tbl = [[k.sb(st1, "H_tb%d_%d" % (i, j), [128, 32, 256], BF16) for j in range(2)] for i in range(2)]
                        ho = [k.sb(st1, "H_ho%d" % i, [128, 512]) for i in range(4)]
                        cnt = 0
                        for kb in range(16):
                            tt = tbl[kb % 2]
                            for br in range(2):
                                k.dma("sp", tt[br].t[:].rearrange("p c n -> p (c n)"), h_tf[br, kb], writes=[tt[br].r])
                            for half in range(2):
                                fc = kb * 2 + half
                                for br in range(2):
                                    pb = PS[cnt % 8]
                                    src = HP if br == 0 else HM

                                    def grp(e, pb=pb, src=src, tt=tt, br=br, half=half):
                                        for sc in range(32):
                                            ins = e.matmul(pb.t[:], lhsT=tt[br].t[:, sc, half * 128:(half + 1) * 128], rhs=src.t[:, sc, :], start=(sc == 0), stop=(sc == 31))
                                        return ins
                                    P.op("pe", grp, reads=[tt[br].r, src.r], writes=[pb.r])
                                    o = ho[cnt % 4]
                                    P.op("dve", lambda e, pb=pb, o=o: e.tensor_tensor(out=o.t[:], in0=pb.t[:], in1=rnorm.t[:], op=ALU.mult), reads=[pb.r, rnorm.r], writes=[o.r])
                                    k.dma("sp", hspec[br, fc * 128:(fc + 1) * 128, :], o.t[:], reads=[o.r])
                                    cnt += 1
                        P.barrier()
                        P.emit()
                with ExitStack() as st:
                    VG = k.sb(st, "H_VG", [128, 32, 512], BF16)
                    with ExitStack() as st1:
                        pin = [k.sb(st1, "H1_in%d" % i, [128, S + 2]) for i in range(2)]
                        uu = [k.sb(st1, "H1_u%d" % i, [128, S]) for i in range(3)]
                        cw = k.sb(st1, "H1_cw", [128, 12, 4])
                        k.dma("sp", cw.t[:].rearrange("p c t -> p (c t)"), h_cw[l], writes=[cw.r])
                        for i in range(2):
                            P.op("pool", lambda e, i=i: e.memset(pin[i].t[:, 0:1], 0.0), writes=[pin[i].r])
                            P.op("pool", lambda e, i=i: e.memset(pin[i].t[:, S + 1:S + 2], 0.0), writes=[pin[i].r])
                        cc = [0]

                        def conv(j, u):
                            pi = pin[cc[0] % 2]
                            cc[0] += 1
                            k.dma("sp", pi.t[:, 1:S + 1], pT[(PC_HY + j) * 128:(PC_HY + j + 1) * 128, :], writes=[pi.r])
                            P.op("dve", lambda e: e.tensor_scalar(out=u.t[:], in0=pi.t[:, 1:S + 1], scalar1=cw.t[:, j, 1:2], scalar2=cw.t[:, j, 3:4], op0=ALU.mult, op1=ALU.add), reads=[pi.r, cw.r], writes=[u.r])
                            P.op("dve", lambda e: e.scalar_tensor_tensor(out=u.t[:], in0=pi.t[:, 0:S], scalar=cw.t[:, j, 0:1], in1=u.t[:], op0=ALU.mult, op1=ALU.add), reads=[pi.r, cw.r, u.r], writes=[u.r])
                            P.op("dve", lambda e: e.scalar_tensor_tensor(out=u.t[:], in0=pi.t[:, 2:S + 2], scalar=cw.t[:, j, 2:3], in1=u.t[:], op0=ALU.mult, op1=ALU.add), reads=[pi.r, cw.r, u.r], writes=[u.r])
                        for jj in range(4):
                            conv(jj, uu[0])
                            k.dma("sp", hx1[jj * 128:(jj + 1) * 128, :], uu[0].t[:], reads=[uu[0].r])
                            conv(4 + jj, uu[1])
                            conv(8 + jj, uu[2])
                            P.op("pool", lambda e: e.tensor_tensor(out=uu[2].t[:], in0=uu[2].t[:], in1=uu[1].t[:], op=ALU.mult), reads=[uu[1].r, uu[2].r], writes=[uu[2].r])
                            k.dma("sp", hvg[jj * 128:(jj + 1) * 128, :], uu[2].t[:], reads=[uu[2].r])
                            for sc in range(32):
                                pb = PS[sc % 8]
                                P.op("pe", lambda e, pb=pb, sc=sc: e.transpose(pb.t[:, 0:128], uu[2].t[:, sc * 128:(sc + 1) * 128], ident.t[:]), reads=[uu[2].r, ident.r], writes=[pb.r])
                                if sc % 2 == 0:
                                    P.op("act", lambda e, pb=pb, sc=sc, jj=jj: e.activation(out=VG.t[:, sc, jj * 128:(jj + 1) * 128], in_=pb.t[:, 0:128], func=AF.Copy), reads=[pb.r], writes=[VG.r])
                                else:
                                    P.op("dve", lambda e, pb=pb, sc=sc, jj=jj: e.tensor_copy(out=VG.t[:, sc, jj * 128:(jj + 1) * 128], in_=pb.t[:, 0:128]), reads=[pb.r], writes=[VG.r])
                        P.barrier()
                        P.emit()
                    YR = k.sb(st, "H_YR", [128, 32, 512], BF16)
                    YI = k.sb(st, "H_YI", [128, 32, 512], BF16)
                    tbl = [[k.sb(st, "H1_tb%d_%d" % (i, j), [128, 32, 256], BF16) for j in range(2)] for i in range(2)]
                    hh = [[k.sb(st, "H1_h%d_%d" % (i, j), [128, 512]) for j in range(2)] for i in range(2)]
                    tq = [k.sb(st, "H1_t%d" % i, [128, 512]) for i in range(4)]
                    skc = k.sb(st, "H1_sk", [128, 4])
                    k.dma("sp", skc.t[:], h_skip[l], writes=[skc.r])
                    cnt = 0
                    for kb in range(16):
                        tt = tbl[kb % 2]
                        for br in range(2):
                            k.dma("sp", tt[br].t[:].rearrange("p c n -> p (c n)"), h_tf[br, kb], writes=[tt[br].r])
                        for half in range(2):
                            fc = kb * 2 + half
                            hq = hh[fc % 2]
                            for br in range(2):
                                k.dma("sp", hq[br].t[:], hspec[br, fc * 128:(fc + 1) * 128, :], writes=[hq[br].r])
                            pbs = []
                            for br in range(2):
                                pb = PS[cnt % 8]
                                cnt += 1

                                def grp(e, pb=pb, tt=tt, br=br, half=half):
                                    for sc in range(32):
                                        ins = e.matmul(pb.t[:], lhsT=tt[br].t[:, sc, half * 128:(half + 1) * 128], rhs=VG.t[:, sc, :], start=(sc == 0), stop=(sc == 31))
                                    return ins
                                P.op("pe", grp, reads=[tt[br].r, VG.r], writes=[pb.r])
                                pbs.append(pb)
                            xr, xi = pbs
                            P.op("dve", lambda e, xr=xr, hq=hq: e.tensor_tensor(out=tq[0].t[:], in0=xr.t[:], in1=hq[0].t[:], op=ALU.mult), reads=[xr.r, hq[0].r], writes=[tq[0].r])
                            P.op("dve", lambda e, xi=xi, hq=hq: e.tensor_tensor(out=tq[1].t[:], in0=xi.t[:], in1=hq[1].t[:], op=ALU.mult), reads=[xi.r, hq[1].r], writes=[tq[1].r])
                            P.op("pool", lambda e, fc=fc: e.tensor_tensor(out=YR.t[:, fc, :], in0=tq[0].t[:], in1=tq[1].t[:], op=ALU.subtract), reads=[tq[0].r, tq[1].r], writes=[YR.r])
                            P.op("dve", lambda e, xr=xr, hq=hq: e.tensor_tensor(out=tq[2].t[:], in0=xr.t[:], in1=hq[1].t[:], op=ALU.mult), reads=[xr.r, hq[1].r], writes=[tq[2].r])
                            P.op("dve", lambda e, xi=xi, hq=hq: e.tensor_tensor(out=tq[3].t[:], in0=xi.t[:], in1=hq[0].t[:], op=ALU.mult), reads=[xi.r, hq[0].r], writes=[tq[3].r])
                            P.op("pool", lambda e, fc=fc: e.tensor_tensor(out=YI.t[:, fc, :], in0=tq[2].t[:], in1=tq[3].t[:], op=ALU.add), reads=[tq[2].r, tq[3].r], writes=[YI.r])
                    vgt = [k.sb(st, "H1_vg%d" % i, [128, 256]) for i in range(2)]
                    x1t = [k.sb(st, "H1_x1%d" % i, [128, 256]) for i in range(2)]
                    ob = [k.sb(st, "H1_ob%d" % i, [128, 256], BF16) for i in range(2)]
                    for tb16 in range(16):
                        tt = tbl[tb16 % 2]
                        tsl = slice(tb16 * 256, (tb16 + 1) * 256)
                        for br in range(2):
                            k.dma("sp", tt[br].t[:].rearrange("p c n -> p (c n)"), h_ti[br, tb16], writes=[tt[br].r])
                        for jj in range(4):
                            pb = PS[cnt % 8]
                            cnt += 1
                            vg_, x1_, o = vgt[cnt % 2], x1t[cnt % 2], ob[cnt % 2]
                            k.dma("sp", vg_.t[:], hvg[jj * 128:(jj + 1) * 128, tsl], writes=[vg_.r])
                            k.dma("sp", x1_.t[:], hx1[jj * 128:(jj + 1) * 128, tsl], writes=[x1_.r])

                            def grp(e, pb=pb, tt=tt, jj=jj):
                                for br in range(2):
                                    Y = YR if br == 0 else YI
                                    for fc in range(32):
                                        ins = e.matmul(pb.t[:, 0:256], lhsT=Y.t[:, fc, jj * 128:(jj + 1) * 128], rhs=tt[br].t[:, fc, :], start=(br == 0 and fc == 0), stop=(br == 1 and fc == 31))
                                return ins
                            P.op("pe", grp, reads=[tt[0].r, tt[1].r, YR.r, YI.r], writes=[pb.r])
                            P.op("dve", lambda e, pb=pb, vg_=vg_, jj=jj: e.scalar_tensor_tensor(out=vg_.t[:], in0=vg_.t[:], scalar=skc.t[:, jj:jj + 1], in1=pb.t[:, 0:256], op0=ALU.mult, op1=ALU.add), reads=[pb.r, vg_.r, skc.r], writes=[vg_.r])
                            P.op("dve", lambda e, vg_=vg_, x1_=x1_, o=o: e.tensor_tensor(out=o.t[:], in0=vg_.t[:], in1=x1_.t[:], op=ALU.mult), reads=[vg_.r, x1_.r], writes=[o.r])
                            k.dma("sp", mixT[1024 + jj * 128:1024 + (jj + 1) * 128, tsl], o.t[:], reads=[o.r])
                    P.barrier()
                    P.emit()

            if "F" in stages:
                with ExitStack() as st:
                    aw = [k.sb(st, "F_aw%d" % i, [128, 32, 512], BF16) for i in range(2)]
                    with ExitStack() as st1:
                        fw32 = k.sb(st1, "F_fw", [128, 4, 128])
                        dc32 = k.sb(st1, "F_dc", [128, 2, 128])
                        wcs = [k.sb(st1, "F_wcs%d" % i, [128, 4, 128], BF16) for i in range(2)]
                        at = [k.sb(st1, "F_a%d" % i, [128, 4, 512], BF16) for i in range(2)]
                        k.dma("sp", fw32.t[:].rearrange("p g e -> p (g e)"), fn_w[l], writes=[fw32.r])
                        k.dma("sp", dc32.t[:], dftc.rearrange("t p c -> p t c"), writes=[dc32.r])
                        for br in range(2):
                            for g in range(4):
                                pb = PS[(br * 4 + g) % 8]
                                P.op("pe", lambda e, pb=pb, br=br, g=g: e.matmul(pb.t[:, 0:128], lhsT=dc32.t[:, br, :], rhs=fw32.t[:, g, :], start=True, stop=True),
                                     reads=[dc32.r, fw32.r], writes=[pb.r])
                                P.op("dve", lambda e, pb=pb, br=br, g=g: e.tensor_copy(out=wcs[br].t[:, g, :], in_=pb.t[:, 0:128]), reads=[pb.r], writes=[wcs[br].r])
                        for sb_ in range(8):
                            a = at[sb_ % 2]
                            k.dma("pool", a.t[:], pT[PC_FN * 128:(PC_FN + 4) * 128, sb_ * 512:(sb_ + 1) * 512].rearrange("(g p) s -> p g s", p=128), writes=[a.r])
                            for s4 in range(4):
                                sc = sb_ * 4 + s4
                                for br in range(2):
                                    pb = PS[(sc * 2 + br) % 8]
                                    for g in range(4):
                                        P.op("pe", lambda e, pb=pb, a=a, g=g, s4=s4, br=br: e.matmul(pb.t[:, g * 128:(g + 1) * 128], lhsT=a.t[:, g, s4 * 128:(s4 + 1) * 128], rhs=wcs[br].t[:, g, :], start=True, stop=True),
                                             reads=[a.r, wcs[br].r], writes=[pb.r])
                                    if br == 0:
                                        P.op("act", lambda e, pb=pb, sc=sc: e.activation(out=aw[0].t[:, sc, :], in_=pb.t[:], func=AF.Copy), reads=[pb.r], writes=[aw[0].r])
                                    else:
                                        P.op("dve", lambda e, pb=pb, sc=sc: e.tensor_copy(out=aw[1].t[:, sc, :], in_=pb.t[:]), reads=[pb.r], writes=[aw[1].r])
                        P.barrier()
                        P.emit()
                    tb_ = [[k.sb(st, "F_t%d_%d" % (i, j), [128, 32, 256], BF16) for j in range(2)] for i in range(2)]
                    ob = [k.sb(st, "F_ob%d" % i, [128, 256], BF16) for i in range(3)]
                    cnt = 0
                    for kb in range(16):
                        tt = tb_[kb % 2]
                        for br in range(2):
                            k.dma("sp", tt[br].t[:].rearrange("p c n -> p (c n)"), fcs[br, kb], writes=[tt[br].r])
                        for j in range(4):
                            pb = PS[cnt % 8]
                            for br in range(2):
                                for sc in range(32):
                                    P.op("pe", lambda e, pb=pb, br=br, sc=sc, j=j, tt=tt: e.matmul(pb.t[:, 0:256], lhsT=aw[br].t[:, sc, j * 128:(j + 1) * 128], rhs=tt[br].t[:, sc, :],
                                                                                          start=(br == 0 and sc == 0), stop=(br == 1 and sc == 31)),
                                         reads=[aw[br].r, tt[br].r], writes=[pb.r])
                            o = ob[cnt % 3]
                            if cnt % 2 == 0:
                                P.op("act", lambda e, pb=pb, o=o: e.activation(out=o.t[:], in_=pb.t[:, 0:256], func=AF.Copy), reads=[pb.r], writes=[o.r])
                            else:
                                P.op("dve", lambda e, pb=pb, o=o: e.tensor_copy(out=o.t[:], in_=pb.t[:, 0:256]), reads=[pb.r], writes=[o.r])
                            k.dma("sp", mixT[1536 + j * 128:1536 + (j + 1) * 128, kb * 256:(kb + 1) * 256], o.t[:], reads=[o.r])
                            cnt += 1
                    P.barrier()
                    P.emit()
            if "E" in stages:
                with ExitStack() as st:
                    mt = k.sb(st, "E_m", [128, KD, TB], BF16)
                    wt = [k.sb(st, "E_w%d" % i, [128, KD, 128], BF16) for i in range(3)]
                    xt = [k.sb(st, "E_x%d" % i, [128, TB]) for i in range(3)]
                    for tb in range(NTB):
                        ts = slice(tb * TB, (tb + 1) * TB)
                        k.dma("sp", mt.t[:], mixT.rearrange("(k p) t -> p k t", p=128)[:, :, ts], writes=[mt.r])
                        for c in range(KD):
                            w = wt[c % 3]
                            x_ = xt[c % 3]
                            k.dma("pool", w.t[:].rearrange("p k c -> p (k c)"), w_out[l, c], writes=[w.r])
                            k.dma("sp", x_.t[:], cur[c * 128:(c + 1) * 128, ts], writes=[x_.r])
                            pb = PS[c % 8]
                            for kk in range(KD):
                                P.op("pe", lambda e, kk=kk, w=w, pb=pb: e.matmul(pb.t[:], lhsT=w.t[:, kk, :], rhs=mt.t[:, kk, :], start=(kk == 0), stop=(kk == KD - 1)),
                                     reads=[w.r, mt.r], writes=[pb.r])
                            P.op("dve", lambda e, x_=x_, pb=pb: e.tensor_tensor(out=x_.t[:], in0=pb.t[:], in1=x_.t[:], op=ALU.add), reads=[pb.r, x_.r], writes=[x_.r])
                            k.dma("sp", x1T[c * 128:(c + 1) * 128, ts], x_.t[:], reads=[x_.r])
                    P.barrier()
                    P.emit()

            if "P" in stages:
                TP = 256
                NTP = S // TP
                with ExitStack() as st:
                    xt = k.sb(st, "P_x", [128, KD, TP])
                    hT = k.sb(st, "P_h", [128, KD, TP], BF16)
                    qT = k.sb(st, "P_q", [128, KD, TP], BF16)
                    sq = [k.sb(st, "P_sq%d" % i, [128, TP]) for i in range(2)]
                    rstd = k.sb(st, "P_rstd", [128, TP])
                    nw = k.sb(st, "P_nw", [128, KD])
                    wt = [k.sb(st, "P_w%d" % i, [128, KD, 128], BF16) for i in range(3)]
                    vt_ = [k.sb(st, "P_v%d" % i, [128, 16, 128], BF16) for i in range(3)]
                    kT = k.sb(st, "P_kT", [128, 2, 128], BF16)
                    iot = k.sb(st, "P_iota", [128, 128])
                    Wt = k.sb(st, "P_W", [128, 128, TP], BF16)
                    scs = k.sb(st, "P_sc", [128, 16, 128])
                    tmp = k.sb(st, "P_tmp", [128, 256])
                    vals = k.sb(st, "P_vals", [128, 16, 16])
                    idx = k.sb(st, "P_idx", [128, 16, 16], U32)
                    idxf = k.sb(st, "P_idxf", [128, 16, 16])
                    cand = k.sb(st, "P_cand", [128, 8, 256])
                    sc16 = k.sb(st, "P_sc16", [128, 8, 16])
                    ci = k.sb(st, "P_ci", [128, 8, 16], U32)
                    cia = k.sb(st, "P_cia", [128, 8, 16], U32)
                    cib = k.sb(st, "P_cib", [128, 8, 16], U32)
                    caf = k.sb(st, "P_caf", [128, 8, 16])
                    cbf = k.sb(st, "P_cbf", [128, 8, 16])
                    e1 = k.sb(st, "P_e1", [128, 8, 16])
                    e2 = k.sb(st, "P_e2", [128, 8, 16])
                    t16 = k.sb(st, "P_t16", [128, 16])
                    gate = k.sb(st, "P_gate", [128, 8, 16])
                    nm = k.sb(st, "P_nm", [128, 8])
                    ssum = k.sb(st, "P_ssum", [128, 8])
                    e1T = k.sb(st, "P_e1T", [128, TP])
                    e2T = k.sb(st, "P_e2T", [128, TP])
                    gT = k.sb(st, "P_gT", [128, TP])
                    Pt = [k.sb(st, "P_Pt%d" % i, [128, 128], BF16) for i in range(4)]
                    Qt = [k.sb(st, "P_Qt%d" % i, [128, 128], BF16) for i in range(4)]
                    gl = [k.sb(st, "P_gl%d" % i, [128, TP], BF16) for i in range(2)]
                    ot = [k.sb(st, "P_o%d" % i, [128, TP]) for i in range(2)]
                    k.dma("sp", nw.t[:], n2[l], writes=[nw.r])
                    k.dma("pool", kT.t[:], p_kT[l].rearrange("s d n -> d s n"), writes=[kT.r])
                    k.dma("sp", iot.t[:], p_iota, writes=[iot.r])
                    pcnt = [0]

                    def nps():
                        pcnt[0] += 1
                        return PS[1 + pcnt[0] % 7]

                    for tb in range(NTP):
                        ts = slice(tb * TP, (tb + 1) * TP)
                        k.dma("sp", xt.t[:], x1T.rearrange("(k p) t -> p k t", p=128)[:, :, ts], writes=[xt.r])
                        rmsnorm(xt, hT, nw, sq, rstd, PS[0], N=TP)
                        for c in range(KD):
                            w = wt[c % 3]
                            k.dma("pool", w.t[:].rearrange("p k c -> p (k c)"), p_wq[l, c], writes=[w.r])
                            pb = nps()
                            for kk in range(KD):
                                P.op("pe", lambda e, kk=kk, w=w, pb=pb: e.matmul(pb.t[:, :TP], lhsT=w.t[:, kk, :], rhs=hT.t[:, kk, :], start=(kk == 0), stop=(kk == KD - 1)),
                                     reads=[w.r, hT.r], writes=[pb.r])
                            P.op("act", lambda e, pb=pb, c=c: e.activation(out=qT.t[:, c, :], in_=pb.t[:, :TP], func=AF.Copy), reads=[pb.r], writes=[qT.r])
                        for tt in range(TP // 128):
                            tsl = slice(tt * 128, (tt + 1) * 128)
                            for c4 in range(4):
                                pb = nps()
                                for cc in range(4):
                                    c = c4 * 4 + cc
                                    P.op("pe", lambda e, pb=pb, c=c, cc=cc, tsl=tsl: e.matmul(pb.t[:, cc * 128:(cc + 1) * 128], lhsT=qT.t[:, c, tsl], rhs=kT.t[:, c % 2, :], start=True, stop=True),
                                         reads=[qT.r, kT.r], writes=[pb.r])
                                P.op("act", lambda e, pb=pb, c4=c4: e.activation(out=scs.t[:, c4 * 4:(c4 + 1) * 4, :].rearrange("p c n -> p (c n)"), in_=pb.t[:], func=AF.Copy), reads=[pb.r], writes=[scs.r])
                            for c in range(16):
                                P.op("dve", lambda e, c=c: e.max(out=vals.t[:, c, 0:8], in_=scs.t[:, c, :]), reads=[scs.r], writes=[vals.r])
                                P.op("dve", lambda e, c=c: e.match_replace(out=tmp.t[:, 0:128], in_to_replace=vals.t[:, c, 0:8], in_values=scs.t[:, c, :], imm_value=-1e30), reads=[vals.r, scs.r], writes=[tmp.r])
                                P.op("dve", lambda e, c=c: e.max(out=vals.t[:, c, 8:16], in_=tmp.t[:, 0:128]), reads=[tmp.r], writes=[vals.r])
                                P.op("dve", lambda e, c=c: e.max_index(out=idx.t[:, c, 0:8], in_max=vals.t[:, c, 0:8], in_values=scs.t[:, c, :]), reads=[vals.r, scs.r], writes=[idx.r])
                                P.op("dve", lambda e, c=c: e.max_index(out=idx.t[:, c, 8:16], in_max=vals.t[:, c, 8:16], in_values=scs.t[:, c, :]), reads=[vals.r, scs.r], writes=[idx.r])
                            P.op("dve", lambda e: e.tensor_copy(out=idxf.t[:], in_=idx.t[:]), reads=[idx.r], writes=[idxf.r])
                            for h in range(8):
                                for a in range(16):
                                    eng = "dve" if a % 2 == 0 else "pool"
                                    P.op(eng, lambda e, h=h, a=a: e.tensor_scalar(out=cand.t[:, h, a * 16:(a + 1) * 16], in0=vals.t[:, 2 * h + 1, :], scalar1=vals.t[:, 2 * h, a:a + 1], scalar2=None, op0=ALU.add),
                                         reads=[vals.r], writes=[cand.r])
                            for h in range(8):
                                P.op("dve", lambda e, h=h: e.max(out=sc16.t[:, h, 0:8], in_=cand.t[:, h, :]), reads=[cand.r], writes=[sc16.r])
                                P.op("dve", lambda e, h=h: e.match_replace(out=tmp.t[:], in_to_replace=sc16.t[:, h, 0:8], in_values=cand.t[:, h, :], imm_value=-1e30), reads=[sc16.r, cand.r], writes=[tmp.r])
                                P.op("dve", lambda e, h=h: e.max(out=sc16.t[:, h, 8:16], in_=tmp.t[:]), reads=[tmp.r], writes=[sc16.r])
                                P.op("dve", lambda e, h=h: e.max_index(out=ci.t[:, h, 0:8], in_max=sc16.t[:, h, 0:8], in_values=cand.t[:, h, :]), reads=[sc16.r, cand.r], writes=[ci.r])
                                P.op("dve", lambda e, h=h: e.max_index(out=ci.t[:, h, 8:16], in_max=sc16.t[:, h, 8:16], in_values=cand.t[:, h, :]), reads=[sc16.r, cand.r], writes=[ci.r])
                            P.op("dve", lambda e: e.tensor_single_scalar(out=cia.t[:], in_=ci.t[:], scalar=4, op=ALU.logical_shift_right), reads=[ci.r], writes=[cia.r])
                            P.op("dve", lambda e: e.tensor_single_scalar(out=cib.t[:], in_=ci.t[:], scalar=15, op=ALU.bitwise_and), reads=[ci.r], writes=[cib.r])
                            P.op("dve", lambda e: e.tensor_copy(out=caf.t[:], in_=cia.t[:]), reads=[cia.r], writes=[caf.r])
                            P.op("dve", lambda e: e.tensor_copy(out=cbf.t[:], in_=cib.t[:]), reads=[cib.r], writes=[cbf.r])
                            P.op("pool", lambda e: e.memset(e1.t[:], 0.0), writes=[e1.r])
                            P.op("pool", lambda e: e.memset(e2.t[:], 0.0), writes=[e2.r])
                            for h in range(8):
                                for a in range(16):
                                    P.op("dve", lambda e, h=h, a=a: e.tensor_scalar(out=t16.t[:], in0=caf.t[:, h, :], scalar1=float(a), scalar2=idxf.t[:, 2 * h, a:a + 1], op0=ALU.is_equal, op1=ALU.mult), reads=[caf.r, idxf.r], writes=[t16.r])
                                    P.op("dve", lambda e, h=h: e.tensor_tensor(out=e1.t[:, h, :], in0=e1.t[:, h, :], in1=t16.t[:], op=ALU.add), reads=[e1.r, t16.r], writes=[e1.r])
                                    P.op("dve", lambda e, h=h, a=a: e.tensor_scalar(out=t16.t[:], in0=cbf.t[:, h, :], scalar1=float(a), scalar2=idxf.t[:, 2 * h + 1, a:a + 1], op0=ALU.is_equal, op1=ALU.mult), reads=[cbf.r, idxf.r], writes=[t16.r])
                                    P.op("dve", lambda e, h=h: e.tensor_tensor(out=e2.t[:, h, :], in0=e2.t[:, h, :], in1=t16.t[:], op=ALU.add), reads=[e2.r, t16.r], writes=[e2.r])
                            P.op("dve", lambda e: e.tensor_scalar(out=nm.t[:], in0=sc16.t[:, :, 0], scalar1=-1.0, scalar2=None, op0=ALU.mult), reads=[sc16.r], writes=[nm.r])
                            for h in range(8):
                                P.op("act", lambda e, h=h: e.activation(out=gate.t[:, h, :], in_=sc16.t[:, h, :], func=AF.Exp, bias=nm.t[:, h:h + 1]), reads=[sc16.r, nm.r], writes=[gate.r])
                            P.op("dve", lambda e: e.tensor_reduce(out=ssum.t[:], in_=gate.t[:], axis=AX.X, op=ALU.add), reads=[gate.r], writes=[ssum.r])
                            P.op("dve", lambda e: e.reciprocal(out=ssum.t[:], in_=ssum.t[:]), reads=[ssum.r], writes=[ssum.r])
                            for h in range(8):
                                P.op("dve", lambda e, h=h: e.tensor_scalar(out=gate.t[:, h, :], in0=gate.t[:, h, :], scalar1=ssum.t[:, h:h + 1], scalar2=None, op0=ALU.mult), reads=[gate.r, ssum.r], writes=[gate.r])
                            for (src, dst) in ((e1, e1T), (e2, e2T), (gate, gT)):
                                pb = nps()
                                P.op("pe", lambda e, pb=pb, src=src: e.transpose(pb.t[:, 0:128], src.t[:].rearrange("p h r -> p (h r)"), ident.t[:]), reads=[src.r, ident.r], writes=[pb.r])
                                P.op("act", lambda e, pb=pb, dst=dst, tsl=tsl: e.activation(out=dst.t[:, tsl], in_=pb.t[:, 0:128], func=AF.Copy), reads=[pb.r], writes=[dst.r])
                        for t4 in range(TP // 4):
                            pb = nps()
                            for q4 in range(4):
                                t = t4 * 4 + q4
                                pt_, qt_ = Pt[q4], Qt[q4]
                                P.op("pool", lambda e, pt_=pt_, t=t: e.tensor_scalar(out=pt_.t[:], in0=iot.t[:], scalar1=e1T.t[:, t:t + 1], scalar2=None, op0=ALU.is_equal), reads=[iot.r, e1T.r], writes=[pt_.r])
                                P.op("dve", lambda e, qt_=qt_, t=t: e.tensor_scalar(out=qt_.t[:], in0=iot.t[:], scalar1=e2T.t[:, t:t + 1], scalar2=gT.t[:, t:t + 1], op0=ALU.is_equal, op1=ALU.mult), reads=[iot.r, e2T.r, gT.r], writes=[qt_.r])
                                P.op("pe", lambda e, pb=pb, pt_=pt_, qt_=qt_, q4=q4: e.matmul(pb.t[:, q4 * 128:(q4 + 1) * 128], lhsT=qt_.t[:], rhs=pt_.t[:], start=True, stop=True), reads=[pt_.r, qt_.r], writes=[pb.r])
                            P.op("act", lambda e, pb=pb, t4=t4: e.activation(out=Wt.t[:, :, t4 * 4:(t4 + 1) * 4], in_=pb.t[:].rearrange("p (t i) -> p i t", t=4), func=AF.Copy), reads=[pb.r], writes=[Wt.r])
                        for ec in range(128):
                            w = wt[ec % 3]
                            k.dma("pool", w.t[:].rearrange("p k c -> p (k c)"), p_uT[l, ec], writes=[w.r])
                            pb = nps()
                            def grp(e, w=w, pb=pb):
                                for kk in range(KD):
                                    ins = e.matmul(pb.t[:, :TP], lhsT=w.t[:, kk, :], rhs=hT.t[:, kk, :], start=(kk == 0), stop=(kk == KD - 1))
                                return ins
                            P.op("pe", grp, reads=[w.r, hT.r], writes=[pb.r])
                            g_ = gl[ec % 2]
                            P.op("act", lambda e, pb=pb, g_=g_: e.activation(out=g_.t[:], in_=pb.t[:, :TP], func=AF.Gelu), reads=[pb.r], writes=[g_.r])
                            P.op("dve", lambda e, g_=g_, ec=ec: e.tensor_tensor(out=Wt.t[:, ec, :], in0=Wt.t[:, ec, :], in1=g_.t[:], op=ALU.mult), reads=[g_.r, Wt.r], writes=[Wt.r])
                        for dc in range(KD):
                            pb = nps()
                            for ig in range(8):
                                v_ = vt_[(dc * 8 + ig) % 3]
                                k.dma("pool", v_.t[:].rearrange("p i d -> p (i d)"), p_v[l, dc, ig], writes=[v_.r])
                                def grp2(e, pb=pb, v_=v_, ig=ig):
                                    for ii in range(16):
                                        i_ = ig * 16 + ii
                                        ins = e.matmul(pb.t[:, :TP], lhsT=v_.t[:, ii, :], rhs=Wt.t[:, i_, :], start=(i_ == 0), stop=(i_ == 127))
                                    return ins
                                P.op("pe", grp2, reads=[v_.r, Wt.r], writes=[pb.r])
                            o = ot[dc % 2]
                            P.op("dve", lambda e, pb=pb, o=o, dc=dc: e.tensor_tensor(out=o.t[:], in0=pb.t[:, :TP], in1=xt.t[:, dc, :], op=ALU.add), reads=[pb.r, xt.r], writes=[o.r])
                            k.dma("sp", x2T[dc * 128:(dc + 1) * 128, ts], o.t[:], reads=[o.r])
                        P.barrier()
                        P.emit()
            cur = x2T if "P" in stages else x1T
        if "N" in stages:
            with ExitStack() as st:
                xt = k.sb(st, "N_x", [128, KD, TB])
                yt = k.sb(st, "N_y", [128, KD, TB])
                sq = [k.sb(st, "N_sq%d" % i, [128, TB]) for i in range(2)]
                rstd = k.sb(st, "N_rstd", [128, TB])
                nw = k.sb(st, "N_nw", [128, KD])
                k.dma("sp", nw.t[:], nf, writes=[nw.r])
                for tb in range(NTB):
                    ts = slice(tb * TB, (tb + 1) * TB)
                    k.dma("sp", xt.t[:], cur.rearrange("(k p) t -> p k t", p=128)[:, :, ts], writes=[xt.r])
                    rmsnorm(xt, yt, nw, sq, rstd, PS[0])
                    k.dma("sp", yT.rearrange("(k p) t -> p k t", p=128)[:, :, ts], yt.t[:], reads=[yt.r])
        P.barrier()
        P.emit()
    return k


def host_prep(inp):
    f = np.float32
    x = np.asarray(inp["x"], f)
    cols = list(range(0, 4096)) + list(range(OFF_HY, IN_COLS))
    w_in = np.asarray(inp["w_in"], f)
    wm = w_in[:, :, cols].reshape(NL, KD, 128, NMAIN, 128).transpose(0, 3, 2, 1, 4).reshape(NL, NMAIN, 128, KD * 128)
    wba = w_in[:, :, OFF_BA:OFF_HY].reshape(NL, KD, 128, 32).transpose(0, 2, 1, 3).reshape(NL, 128, KD * 32)
    vec = lambda v: np.ascontiguousarray(np.asarray(v, f).reshape(-1, KD, 128).transpose(0, 2, 1))
    shared = {
        "n1": vec(inp["norm1_w"]), "n2": vec(inp["norm2_w"]), "nf": vec(inp["final_norm_w"])[0],
        "w_main": np.ascontiguousarray(wm), "w_ba": np.ascontiguousarray(wba),
    }
    w_out = np.asarray(inp["w_out"], f)
    shared["w_out"] = np.ascontiguousarray(w_out.reshape(NL, KD, 128, KD, 128).transpose(0, 3, 2, 1, 4).reshape(NL, KD, 128, KD * 128))
    shared["fn_w"] = np.ascontiguousarray(np.asarray(inp["fnet_w"], f).transpose(0, 2, 1, 3).reshape(NL, 128, 512))
    cwv = np.asarray(inp["gdn_conv_w"], f)
    shared["g_cw"] = np.ascontiguousarray(cwv.reshape(NL, 3, 24, 128).transpose(0, 3, 2, 1).reshape(NL, 128, 72))
    par = np.stack([np.asarray(inp["gdn_a_log"], f), np.asarray(inp["gdn_dt_bias"], f)], axis=1)
    par = np.broadcast_to(par[:, None, :, :, None, :], (NL, 128, 2, 2, 32, 8))
    shared["g_par"] = np.ascontiguousarray(par).reshape(NL, 128, 1024)
    shared["g_nw"] = np.ascontiguousarray(np.broadcast_to(np.asarray(inp["gdn_norm_w"], f)[:, None, :], (NL, 128, 128)))
    wq = np.asarray(inp["peer_wq"], f)
    shared["p_wq"] = np.ascontiguousarray(wq.reshape(NL, KD, 128, KD, 128).transpose(0, 3, 2, 1, 4).reshape(NL, KD, 128, KD * 128))
    shared["p_kT"] = np.ascontiguousarray(np.stack([np.asarray(inp["peer_k1"], f).transpose(0, 2, 1), np.asarray(inp["peer_k2"], f).transpose(0, 2, 1)], axis=1))
    shared["p_iota"] = np.ascontiguousarray(np.broadcast_to(np.arange(128, dtype=f)[None, :], (128, 128)))
    u = np.asarray(inp["peer_u"], f)
    shared["p_uT"] = np.ascontiguousarray(u.reshape(NL, 128, 128, KD, 128).transpose(0, 1, 4, 3, 2).reshape(NL, 128, 128, KD * 128))
    v = np.asarray(inp["peer_v"], f)
    shared["p_v"] = np.ascontiguousarray(v.reshape(NL, 8, 16, 128, KD, 128).transpose(0, 4, 1, 3, 2, 5).reshape(NL, KD, 8, 128, 16 * 128))
    shared["h_w1"] = np.ascontiguousarray(np.asarray(inp["hy_w1"], f))
    shared["h_w23"] = np.ascontiguousarray(np.stack([np.asarray(inp["hy_w2"], f), np.asarray(inp["hy_w3"], f)], axis=1))
    shared["h_w4b"] = np.ascontiguousarray(np.concatenate([np.asarray(inp["hy_w4"], f), np.asarray(inp["hy_b4"], f)[:, None, :]], axis=1))
    shared["h_bf"] = np.ascontiguousarray(np.stack([np.asarray(inp[n_], f) for n_ in ("hy_b1", "hy_b2", "hy_b3", "hy_freq")], axis=2))
    hcw = np.concatenate([np.asarray(inp["hy_conv_w"], f), np.asarray(inp["hy_conv_b"], f)[:, None, :]], axis=1)
    shared["h_cw"] = np.ascontiguousarray(hcw.reshape(NL, 4, 12, 128).transpose(0, 3, 2, 1).reshape(NL, 128, 48))
    shared["h_skip"] = np.ascontiguousarray(np.asarray(inp["hy_skip"], f).reshape(NL, 4, 128).transpose(0, 2, 1))
    shared.update(const_tables())
    maps = []
    for b in range(8):
        m = dict(shared)
        m["xT"] = np.ascontiguousarray(x[b].T)
        maps.append(m)
    return maps


_CONST = None


def tbl_layout(t):
    return np.ascontiguousarray(t.reshape(32, 128, 16, 256).transpose(2, 1, 0, 3).reshape(16, 128, 32 * 256))


def const_tables():
    global _CONST
    if _CONST is not None:
        return _CONST
    bf = ml_dtypes.bfloat16
    c = {}
    i128 = np.arange(128)
    ang = 2 * np.pi * ((i128[:, None] * i128[None, :]) % 128) / 128
    c["dftc"] = np.stack([np.cos(ang), np.sin(ang)]).astype(np.float32)
    ii, jj = i128[:, None], i128[None, :]
    NEG = -1.0e30
    gm = np.zeros((6, 128, 128), np.float32)
    gm[0] = np.where(ii > jj, 0.0, NEG)
    gm[1] = np.where(jj > ii, 0.0, NEG)
    gm[2] = np.where(jj >= ii, 0.0, NEG)
    gm[3] = np.where(ii >= jj, 0.0, NEG)
    gm[4] = (ii <= jj)
    gm[5] = (ii >= jj)
    c["gmask"] = gm
    sel = np.zeros((8, 8, 128), np.float32)
    for h in range(8):
        sel[h, h, :] = 1.0
    c["gsel"] = sel.reshape(8, 1024)
    n = np.arange(S, dtype=np.int64)
    prod = (n[:, None] * n[None, :]) % S
    ang = (2 * np.pi / S) * prod.astype(np.float64)
    sc = 1.0 / np.sqrt(S * 128.0)
    c["fcs"] = np.stack([tbl_layout((np.cos(ang) * sc).astype(np.float32)).astype(bf),
                         tbl_layout((-np.sin(ang) * sc).astype(np.float32)).astype(bf)])
    import math
    t = np.linspace(0.0, 1.0, S, dtype=np.float32)[:, None]
    ang = (np.float32(2.0 * math.pi / S) * np.arange(S, dtype=np.float32))[:, None]
    fq = np.linspace(1e-4, 15, 16, dtype=np.float32)[None, :]
    zz = np.concatenate([t, np.cos(fq * ang), -np.sin(fq * ang)], axis=-1).astype(np.float32)
    c["h_z"] = np.ascontiguousarray(zz.T)
    deltas = np.linspace(math.log(1e-2) / 1.5, math.log(1e-2) / 0.3, 512, dtype=np.float32)
    c["h_win"] = np.exp(-t * np.abs(deltas)[None, :]).astype(np.float32).reshape(32, 128, 512)
    M = 2 * S
    prod2 = (n[:, None] * (2 * n[None, :] + 1)) % (2 * M)
    ang2 = (np.pi / M) * prod2.astype(np.float64)
    cf = np.cos(ang2).astype(np.float32)
    sf = (-np.sin(ang2)).astype(np.float32)
    c["h_tf"] = np.stack([tbl_layout(cf).astype(bf), tbl_layout(sf).astype(bf)])
    sc2 = np.float32(2.0 / M)
    c["h_ti"] = np.stack([tbl_layout(np.ascontiguousarray(cf.T) * sc2).astype(bf), tbl_layout(np.ascontiguousarray(sf.T) * sc2).astype(bf)])
    _CONST = c
    return c


def kernel(**inputs):
    k = build()
    maps = host_prep(inputs)
    names = [n for n in k.dram if n in maps[0]]
    in_maps = [{n: m[n] for n in names} for m in maps]
    res = run_bass_kernel_spmd(k.nc, in_maps, core_ids=list(range(8)))
    out = np.stack([np.ascontiguousarray(np.asarray(r["yT"]).T) for r in res.results])
    return out.astype(np.float32)
```
